# Optimizing a Trainium2 kernel written in Bass

```python
import math
import jax, jax.numpy as jnp
from jax import lax
import numpy as np

D_MODEL = 1024
BATCH = 2
SEQ = 8192
DEPTH = 1

ATTN_HEADS = 8
ATTN_QK_DIM = 64
ATTN_V_DIM = 2 * ATTN_QK_DIM
ATTN_QK_COLS = ATTN_HEADS * 2 * ATTN_QK_DIM
ATTN_WIDTH = ATTN_HEADS * ATTN_V_DIM
ROPE_DIM = ATTN_QK_DIM // 4
ROPE_THETA = 500000.0
Q_BLOCK = 128
SSD_INNER = 2 * D_MODEL
SSD_HEAD_DIM = 64
SSD_HEADS = SSD_INNER // SSD_HEAD_DIM
SSD_GROUPS = 4
SSD_STATE = 128
SSD_CONV = 4
SSD_CHUNK = 128
SSD_CONV_CH = SSD_INNER + 2 * SSD_GROUPS * SSD_STATE
N_EXPERTS = 256
TOP_K = 8
N_EXPERT_GROUPS = 8
TOPK_GROUPS = 4
EXPERT_FF = 256
SHARED_FF = 256
ROUTED_SCALE = 2.5
MOE_BLOCK = 128
NORM_EPS = 1e-6
IN_SIZES = (ATTN_QK_COLS, ATTN_QK_COLS, ATTN_WIDTH, SSD_INNER, SSD_CONV_CH, SSD_HEADS, 2 * D_MODEL)
IN_COLS = int(sum(IN_SIZES))
IN_SPLITS = [int(s) for s in np.cumsum(IN_SIZES)[:-1]]

kernel_name = 'hybrid_diffattn_ssd_moe_block'


def rms_norm(x, gain):
    xf = x.astype(jnp.float32)
    y = xf * lax.rsqrt(jnp.mean(xf * xf, axis=-1, keepdims=True) + NORM_EPS)
    return (y * gain.astype(jnp.float32)).astype(x.dtype)


def partial_rope(t, positions):
    half = ROPE_DIM // 2
    inv_freq = 1.0 / (ROPE_THETA ** (jnp.arange(0, ROPE_DIM, 2, dtype=jnp.float32) / ROPE_DIM))
    ang = positions.astype(jnp.float32)[..., None] * inv_freq
    cos = jnp.cos(ang)[:, :, None, None, :]
    sin = jnp.sin(ang)[:, :, None, None, :]
    r1 = t[..., :half].astype(jnp.float32)
    r2 = t[..., half:ROPE_DIM].astype(jnp.float32)
    rot = jnp.concatenate([r1 * cos - r2 * sin, r2 * cos + r1 * sin], axis=-1).astype(t.dtype)
    return jnp.concatenate([rot, t[..., ROPE_DIM:]], axis=-1)


def diff_attention(q, k, v, lam, head_gain, lambda_init):
    bsz, seq = q.shape[0], q.shape[1]
    nq = seq // Q_BLOCK
    scale = ATTN_QK_DIM ** -0.5
    qb = jnp.moveaxis(q.reshape(bsz, nq, Q_BLOCK, ATTN_HEADS, 2, ATTN_QK_DIM), 1, 0)
    key_pos = jnp.arange(seq)

    def one_block(args):
        q_blk, blk = args
        s = jnp.einsum('bqhcd,bkhcd->bhcqk', q_blk, k, preferred_element_type=jnp.float32) * scale
        q_pos = blk * Q_BLOCK + jnp.arange(Q_BLOCK)
        mask = key_pos[None, :] <= q_pos[:, None]
        p = jax.nn.softmax(jnp.where(mask, s, -jnp.inf), axis=-1)
        a = p[:, :, 0] - lam * p[:, :, 1]
        return jnp.einsum('bhqk,bkhd->bqhd', a.astype(v.dtype), v)

    o = lax.map(one_block, (qb, jnp.arange(nq)))
    o = jnp.moveaxis(o, 0, 1).reshape(bsz, seq, ATTN_HEADS, ATTN_V_DIM)
    o = rms_norm(o, head_gain) * (1.0 - lambda_init)
    return o.reshape(bsz, seq, ATTN_WIDTH)


def causal_depthwise_conv(u, w, b):
    out = lax.conv_general_dilated(u, w[:, None, :], window_strides=(1,), padding=[(SSD_CONV - 1, 0)],
                                   dimension_numbers=('NWC', 'WIO', 'NWC'), feature_group_count=u.shape[-1])
    return out + b


def ssd_scan(xh, dt, a_neg, b_in, c_in):
    bsz, seq = xh.shape[0], xh.shape[1]
    nc = seq // SSD_CHUNK
    e = SSD_HEADS // SSD_GROUPS

    def chunks(t):
        return jnp.moveaxis(t.reshape(bsz, nc, SSD_CHUNK, *t.shape[2:]), 1, 0)

    xdt = (xh.astype(jnp.float32) * dt[..., None]).reshape(bsz, seq, SSD_GROUPS, e, SSD_HEAD_DIM)
    a = (dt * a_neg).reshape(bsz, seq, SSD_GROUPS, e)
    causal = jnp.tril(jnp.ones((SSD_CHUNK, SSD_CHUNK), dtype=bool))

    def step(state, inp):
        x_c, a_c, b_c, c_c = inp
        a_cum = lax.cumsum(a_c, axis=1)
        seg = a_cum[:, :, None] - a_cum[:, None, :]
        decay = jnp.exp(jnp.where(causal[None, :, :, None, None], seg, -jnp.inf))
        cb = jnp.einsum('blgn,bsgn->blsg', c_c, b_c)
        y_diag = jnp.einsum('blsg,blsge,bsgep->blgep', cb, decay, x_c)
        y_off = jnp.einsum('blgn,bgepn,blge->blgep', c_c, state, jnp.exp(a_cum))
        to_end = jnp.exp(a_cum[:, -1:] - a_cum)
        new_state = (state * jnp.exp(a_cum[:, -1])[..., None, None]
                     + jnp.einsum('blgn,blge,blgep->bgepn', b_c, to_end, x_c))
        return new_state, y_diag + y_off

    state0 = jnp.zeros((bsz, SSD_GROUPS, e, SSD_HEAD_DIM, SSD_STATE), jnp.float32)
    _, y = lax.scan(step, state0, (chunks(xdt), chunks(a), chunks(b_in.astype(jnp.float32)),
                                   chunks(c_in.astype(jnp.float32))))
    return jnp.moveaxis(y, 0, 1).reshape(bsz, seq, SSD_HEADS, SSD_HEAD_DIM)


def route(h, w_router, router_bias):
    n = h.shape[0]
    scores = jax.nn.sigmoid(jnp.einsum('nd,de->ne', h, w_router, preferred_element_type=jnp.float32))
    choice = scores + router_bias.astype(jnp.float32)
    per_group = N_EXPERTS // N_EXPERT_GROUPS
    group_score = lax.top_k(choice.reshape(n, N_EXPERT_GROUPS, per_group), 2)[0].sum(-1)
    _, top_groups = lax.top_k(group_score, TOPK_GROUPS)
    group_mask = jnp.any(top_groups[..., None] == jnp.arange(N_EXPERT_GROUPS), axis=-2)
    masked = jnp.where(jnp.repeat(group_mask, per_group, axis=-1), choice, -jnp.inf)
    _, idx = lax.top_k(masked, TOP_K)
    w = jnp.take_along_axis(scores, idx, axis=-1)
    w = w / (jnp.sum(w, axis=-1, keepdims=True) + 1e-20) * ROUTED_SCALE
    return idx, w


def routed_experts(h, idx, wts, w_gate, w_up, w_down):
    n, d = h.shape
    n_pairs = n * TOP_K
    n_blocks = -(-n_pairs // MOE_BLOCK) + N_EXPERTS
    flat_e = idx.reshape(-1)
    flat_t = jnp.arange(n_pairs, dtype=jnp.int32) // TOP_K
    flat_w = wts.reshape(-1)
    order = jnp.argsort(flat_e)
    e_sorted = flat_e[order]
    counts = jnp.bincount(flat_e, length=N_EXPERTS)
    padded = (counts + MOE_BLOCK - 1) // MOE_BLOCK * MOE_BLOCK
    padded_end = jnp.cumsum(padded)
    padded_start = padded_end - padded
    start = jnp.cumsum(counts) - counts
    dest = padded_start[e_sorted] + jnp.arange(n_pairs) - start[e_sorted]
    slots = n_blocks * MOE_BLOCK
    slot_tok = jnp.full((slots,), n, jnp.int32).at[dest].set(flat_t[order])
    slot_w = jnp.zeros((slots,), h.dtype).at[dest].set(flat_w[order].astype(h.dtype))
    block_e = jnp.minimum(jnp.searchsorted(padded_end, jnp.arange(n_blocks) * MOE_BLOCK, side='right'),
                          N_EXPERTS - 1)
    h_pad = jnp.concatenate([h, jnp.zeros((1, d), h.dtype)], axis=0)

    def step(acc, inp):
        tok, wt, e = inp
        xb = h_pad[tok]
        y = (jax.nn.silu(xb @ w_gate[e]) * (xb @ w_up[e])) @ w_down[e]
        return acc.at[tok].add(y * wt[:, None]), None

    acc, _ = lax.scan(step, jnp.zeros((n + 1, d), h.dtype),
                      (slot_tok.reshape(n_blocks, MOE_BLOCK), slot_w.reshape(n_blocks, MOE_BLOCK), block_e))
    return acc[:n]


def swiglu(h, wg, wu, wd):
    return (jax.nn.silu(h @ wg) * (h @ wu)) @ wd


def setup_inputs(seed: int = 0) -> dict:
    key = jax.random.key(seed)
    ks = jax.random.split(key, 32)
    f32 = jnp.float32
    L = DEPTH

    def nrm(k, shape, scale):
        return jax.random.normal(k, shape, f32) * scale

    dt0 = jnp.exp(jax.random.uniform(ks[14], (L, SSD_HEADS), f32, math.log(1e-3), math.log(1e-1)))
    return {
        'x': nrm(ks[0], (BATCH, SEQ, D_MODEL), 1.0),
        'c': nrm(ks[1], (BATCH, D_MODEL), 1.0),
        'positions': jnp.broadcast_to(jnp.arange(SEQ, dtype=jnp.int32), (BATCH, SEQ)),
        'w_ada': nrm(ks[2], (L, D_MODEL, 6 * D_MODEL), 0.5 * D_MODEL ** -0.5),
        'b_ada': nrm(ks[3], (L, 6 * D_MODEL), 0.02),
        'norm_mix': 1.0 + nrm(ks[4], (L, D_MODEL), 0.02),
        'w_in': nrm(ks[5], (L, D_MODEL, IN_COLS), D_MODEL ** -0.5),
        'b_gate': nrm(ks[6], (L, 2 * D_MODEL), 0.02),
        'lambda_q1': nrm(ks[7], (L, ATTN_QK_DIM), 0.1),
        'lambda_k1': nrm(ks[8], (L, ATTN_QK_DIM), 0.1),
        'lambda_q2': nrm(ks[9], (L, ATTN_QK_DIM), 0.1),
        'lambda_k2': nrm(ks[10], (L, ATTN_QK_DIM), 0.1),
        'attn_head_norm': 1.0 + nrm(ks[11], (L, ATTN_V_DIM), 0.02),
        'conv_w': nrm(ks[12], (L, SSD_CONV, SSD_CONV_CH), SSD_CONV ** -0.5),
        'conv_b': nrm(ks[13], (L, SSD_CONV_CH), 0.02),
        'dt_bias': dt0 + jnp.log(-jnp.expm1(-dt0)),
        'a_log': jnp.log(jax.random.uniform(ks[15], (L, SSD_HEADS), f32, 1.0, 16.0)),
        'd_skip': 1.0 + nrm(ks[16], (L, SSD_HEADS), 0.02),
        'ssd_norm': 1.0 + nrm(ks[17], (L, SSD_INNER), 0.02),
        'w_branch_attn': nrm(ks[18], (L, ATTN_WIDTH, D_MODEL), ATTN_WIDTH ** -0.5),
        'w_branch_ssd': nrm(ks[19], (L, SSD_INNER, D_MODEL), SSD_INNER ** -0.5),
        'w_out': nrm(ks[20], (L, D_MODEL, D_MODEL), D_MODEL ** -0.5),
        'norm_ffn': 1.0 + nrm(ks[21], (L, D_MODEL), 0.02),
        'w_router': nrm(ks[22], (L, D_MODEL, N_EXPERTS), D_MODEL ** -0.5),
        'router_bias': nrm(ks[23], (L, N_EXPERTS), 0.01),
        'w_exp_gate': nrm(ks[24], (L, N_EXPERTS, D_MODEL, EXPERT_FF), D_MODEL ** -0.5),
        'w_exp_up': nrm(ks[25], (L, N_EXPERTS, D_MODEL, EXPERT_FF), D_MODEL ** -0.5),
        'w_exp_down': nrm(ks[26], (L, N_EXPERTS, EXPERT_FF, D_MODEL), EXPERT_FF ** -0.5),
        'w_sh_gate': nrm(ks[27], (L, D_MODEL, SHARED_FF), D_MODEL ** -0.5),
        'w_sh_up': nrm(ks[28], (L, D_MODEL, SHARED_FF), D_MODEL ** -0.5),
        'w_sh_down': nrm(ks[29], (L, SHARED_FF, D_MODEL), SHARED_FF ** -0.5),
        'norm_final': 1.0 + nrm(ks[30], (D_MODEL,), 0.02),
    }


def reference(x, c, positions, w_ada, b_ada, norm_mix, w_in, b_gate, lambda_q1, lambda_k1, lambda_q2,
              lambda_k2, attn_head_norm, conv_w, conv_b, dt_bias, a_log, d_skip, ssd_norm, w_branch_attn,
              w_branch_ssd, w_out, norm_ffn, w_router, router_bias, w_exp_gate, w_exp_up, w_exp_down,
              w_sh_gate, w_sh_up, w_sh_down, norm_final):
    bsz, seq, d = x.shape
    for l in range(DEPTH):
        mod = jax.nn.silu(c) @ w_ada[l] + b_ada[l]
        shift_m, scale_m, gate_m, shift_f, scale_f, gate_f = jnp.split(mod[:, None, :], 6, axis=-1)

        h = rms_norm(x, norm_mix[l]) * (1.0 + scale_m) + shift_m
        proj = h @ w_in[l]
        q, k, v, z, xbc, dt_raw, gates = jnp.split(proj, IN_SPLITS, axis=-1)

        q = partial_rope(q.reshape(bsz, seq, ATTN_HEADS, 2, ATTN_QK_DIM), positions)
        k = partial_rope(k.reshape(bsz, seq, ATTN_HEADS, 2, ATTN_QK_DIM), positions)
        v = v.reshape(bsz, seq, ATTN_HEADS, ATTN_V_DIM)
        lambda_init = 0.8 - 0.6 * math.exp(-0.3 * l)
        lam = (jnp.exp(jnp.sum(lambda_q1[l].astype(jnp.float32) * lambda_k1[l].astype(jnp.float32)))
               - jnp.exp(jnp.sum(lambda_q2[l].astype(jnp.float32) * lambda_k2[l].astype(jnp.float32)))
               + lambda_init)
        y_attn = diff_attention(q, k, v, lam, attn_head_norm[l], lambda_init)

        xbc = jax.nn.silu(causal_depthwise_conv(xbc, conv_w[l], conv_b[l]))
        xs, b_in, c_in = jnp.split(xbc, [SSD_INNER, SSD_INNER + SSD_GROUPS * SSD_STATE], axis=-1)
        dt = jax.nn.softplus(dt_raw.astype(jnp.float32) + dt_bias[l].astype(jnp.float32))
        a_neg = -jnp.exp(a_log[l].astype(jnp.float32))
        xh = xs.reshape(bsz, seq, SSD_HEADS, SSD_HEAD_DIM)
        y = ssd_scan(xh, dt, a_neg, b_in.reshape(bsz, seq, SSD_GROUPS, SSD_STATE),
                     c_in.reshape(bsz, seq, SSD_GROUPS, SSD_STATE))
        y = y + d_skip[l].astype(jnp.float32)[:, None] * xh.astype(jnp.float32)
        y = y.reshape(bsz, seq, SSD_INNER) * jax.nn.silu(z.astype(jnp.float32))
        y = rms_norm(y.reshape(bsz, seq, SSD_GROUPS, SSD_INNER // SSD_GROUPS),
                     ssd_norm[l].reshape(SSD_GROUPS, SSD_INNER // SSD_GROUPS))
        y_ssd = y.reshape(bsz, seq, SSD_INNER).astype(x.dtype)

        g_attn, g_ssd = jnp.split(jax.nn.sigmoid(gates + b_gate[l]), 2, axis=-1)
        mixed = g_attn * (y_attn @ w_branch_attn[l]) + g_ssd * (y_ssd @ w_branch_ssd[l])
        x = x + gate_m * (mixed @ w_out[l])

        h2 = rms_norm(x, norm_ffn[l]) * (1.0 + scale_f) + shift_f
        flat = h2.reshape(bsz * seq, d)
        idx, wts = route(flat, w_router[l], router_bias[l])
        routed = routed_experts(flat, idx, wts, w_exp_gate[l], w_exp_up[l], w_exp_down[l])
        shared = swiglu(flat, w_sh_gate[l], w_sh_up[l], w_sh_down[l])
        x = x + gate_f * (routed + shared).reshape(bsz, seq, d)
    return rms_norm(x, norm_final)
```

```python
import contextlib
import math
import numpy as np
import ml_dtypes
import concourse.bass as bass
import concourse.mybir as mybir
from concourse.bass_utils import run_bass_kernel_spmd

F32 = mybir.dt.float32
BF16 = mybir.dt.bfloat16
I32 = mybir.dt.int32
AF = mybir.ActivationFunctionType
ALU = mybir.AluOpType
AX = mybir.AxisListType

ENGS = ["pe", "act", "dve", "pool", "sp"]
NDMA = 24
EPS = 1e-6
TWO_PI = 6.283185307179586
NEXP = 256
DEBUG = False
import os
SUB = int(os.environ.get('KSUB', '9'))


class Prog:
    def __init__(self, nc):
        self.nc = nc
        self.ops = {e: [] for e in ENGS}
        self.count = {e: 0 for e in ENGS}
        self.last_w = {}
        self.readers = {}
        self.waited = {e: {} for e in ENGS}
        self.pending = {e: [] for e in ENGS}
        self.dma_i = 0
        self.dma_last = {}
        self.out_deps = []
        self.enabled = True

    def _need(self, eng, dep, waits):
        if dep is None:
            return
        k, v = dep
        if k == "pe" and eng == "pe":
            return
        if self.waited[eng].get(k, 0) >= v:
            return
        self.waited[eng][k] = v
        waits.append((k, v))

    def barrier(self):
        deps = [(e, self.count[e]) for e in ENGS if self.count[e] > 0]
        deps += [(k, v) for k, v in self.dma_last.items()]
        for e in ENGS:
            self.pending[e] = list(deps)

    def op(self, eng, fn, reads=(), writes=(), dma=False, is_out=False):
        if not self.enabled:
            return None
        waits = []
        for d in self.pending[eng]:
            self._need(eng, d, waits)
        self.pending[eng] = []
        for k in reads:
            self._need(eng, self.last_w.get(k), waits)
        for k in writes:
            self._need(eng, self.last_w.get(k), waits)
            for d in self.readers.get(k, ()):
                self._need(eng, d, waits)
        if dma:
            s = self.dma_i % NDMA
            n = self.dma_i // NDMA
            if n > 0:
                self._need(eng, (("dma", s), 16 * n), waits)
            dep = (("dma", s), 16 * (n + 1))
            inc = (("dma", s), 16)
            self.dma_last[("dma", s)] = 16 * (n + 1)
            self.dma_i += 1
        else:
            self.count[eng] += 1
            dep = (eng, self.count[eng])
            inc = (eng, 1)
        m = {}
        for k, v in waits:
            m[k] = max(m.get(k, 0), v)
        self.ops[eng].append((list(m.items()), fn, inc))
        for k in reads:
            self.readers.setdefault(k, []).append(dep)
        for k in writes:
            self.last_w[k] = dep
            self.readers[k] = []
        if is_out:
            self.out_deps.append(dep)
        return dep

    def emit(self):
        nc = self.nc
        waits = []
        for d in self.out_deps:
            self._need("sp", d, waits)
        m = {}
        for k, v in waits:
            m[k] = max(m.get(k, 0), v)
        final_waits = list(m.items())
        with contextlib.ExitStack() as st:
            sems = {}
            for e in ENGS:
                sems[e] = st.enter_context(nc.semaphore("s_" + e))
            for i in range(NDMA):
                sems[("dma", i)] = st.enter_context(nc.semaphore("s_dma%d" % i))
            block = st.enter_context(nc.Block())

            def run(engname):
                def body(eng):
                    for waits_, fn, inc in self.ops[engname]:
                        for k, v in waits_:
                            eng.wait_ge(sems[k], v)
                        ins = fn(eng)
                        ins.then_inc(sems[inc[0]], inc[1])
                    if engname == "sp":
                        for k, v in final_waits:
                            eng.wait_ge(sems[k], v)
                return body

            block.tensor(run("pe"))
            block.scalar(run("act"))
            block.vector(run("dve"))
            block.gpsimd(run("pool"))
            block.sync(run("sp"))


def build_nc(n_exp=NEXP, debug=False, upto=99, only=None):
    nc = bass.Bass("TRN2", target_bir_lowering=False)
    P = Prog(nc)
    op = P.op

    def din(name, shape, dt=F32):
        return nc.dram_tensor(name, list(shape), dt, kind="ExternalInput").ap()

    def dscr(name, shape, dt=F32):
        return nc.dram_tensor(name, list(shape), dt).ap()

    xl = din("xl", [8192, 1024])
    validb = din("validb", [128, 8192])
    valid_tm_d = din("valid_tm", [128, 64])
    posb = din("posb", [128, 8192], I32)
    c_col = din("c_col", [128, 8])
    w_ada = din("w_ada", [1024, 6144])
    b_ada = din("b_ada", [1, 6144])
    norm_mix = din("norm_mix", [1, 1024])
    norm_ffn = din("norm_ffn", [1, 1024])
    norm_final = din("norm_final", [1, 1024])
    wqk = din("wqk", [8, 5, 1024, 128])
    wz_d = din("wz", [1024, 2048])
    wxbc_d = din("wxbc", [1024, 3072])
    wdt_d = din("wdt", [1024, 32])
    wg_d = din("wg", [1024, 2048])
    b_gate = din("b_gate", [1, 2048])
    lam_d = din("lamv", [4, 64])
    ahn = din("ahn", [1, 128])
    convw_d = din("convw", [128, 24, 4])
    convb_d = din("convb", [128, 24])
    dtb_d = din("dt_bias", [1, 32])
    alog_d = din("a_log", [1, 32])
    dskip_d = din("d_skip", [1, 32])
    ssdn_d = din("ssd_norm", [1, 2048])
    wba_d = din("wba", [1024, 1024])
    wbs_d = din("wbs", [2048, 1024])
    wo_d = din("wo", [1024, 1024])
    wr_d = din("wr", [1024, 256])
    rbias_d = din("rbias", [1, 256])
    weg = din("weg", [n_exp + 1, 1024, 256])
    weu = din("weu", [n_exp + 1, 1024, 256])
    wed = din("wed", [n_exp + 1, 256, 1024])
    ident_d = din("ident", [128, 128])
    tri_d = din("tri", [128, 128])
    ones_d = din("ones", [128, 128])
    cmask_d = din("cmask", [4, 128, 512])
    invf_d = din("invf", [128, 2])
    y_out = nc.dram_tensor("y_out", [2048, 1024], F32, kind="ExternalOutput").ap()

    modrow = dscr("modrow", [1, 6144])
    hT_d = dscr("hT_d", [128, 8, 8192], BF16)
    ropeC = dscr("ropeC", [128, 8192])
    ropeS = dscr("ropeS", [128, 8192])
    yaT_d = dscr("yaT_d", [128, 8, 2048], BF16)
    ypre_d = dscr("ypre_d", [2048, 2048])
    ps_d = dscr("ps_d", [2048, 1024])
    x1_d = dscr("x1_d", [2048, 1024])

    dbg = {}

    with contextlib.ExitStack() as top:
        _cnt = [0]

        def SB(st, name, shape, dt=F32):
            _cnt[0] += 1
            return st.enter_context(nc.sbuf_tensor("sb%d_%s" % (_cnt[0], name), list(shape), dt))

        ps = top.enter_context(nc.psum_tensor("ps", [128, 8, 512], F32))
        ident = SB(top, "ident", [128, 128])
        tri = SB(top, "tri", [128, 128])
        ones = SB(top, "ones", [128, 128])
        vtm = SB(top, "vtm", [128, 64])
        small = SB(top, "small", [128, 64])
        op("sp", lambda e: e.dma_start(out=ident[:], in_=ident_d), writes=["ident"], dma=True)
        op("sp", lambda e: e.dma_start(out=tri[:], in_=tri_d), writes=["tri"], dma=True)
        op("sp", lambda e: e.dma_start(out=ones[:], in_=ones_d), writes=["ones"], dma=True)
        op("sp", lambda e: e.dma_start(out=vtm[:], in_=valid_tm_d), writes=["vtm"], dma=True)

        def bank(i):
            return ps[:, i, :]

        def bcast_load(dst, src_row, key):
            op("sp", lambda e: e.dma_start(out=dst, in_=src_row.partition_broadcast(128)), writes=[key], dma=True)

        def rstd_from_ss(ss_ap, n, out_ap, tmp_ap, keys):
            op("dve", lambda e: e.tensor_scalar(out=tmp_ap, in0=ss_ap, scalar1=1.0 / n, scalar2=EPS, op0=ALU.mult, op1=ALU.add),
               reads=keys, writes=keys)
            op("act", lambda e: e.sqrt(out=tmp_ap, in_=tmp_ap), reads=keys, writes=keys)
            op("dve", lambda e: e.reciprocal(out=out_ap, in_=tmp_ap), reads=keys, writes=keys)

        P.enabled = only is None
        with contextlib.ExitStack() as st:
            cc_t = SB(st, "cc_t", [128, 8])
            sc_t = SB(st, "sc_t", [128, 8])
            modr = SB(st, "modr", [1, 6144])
            bada = SB(st, "bada", [1, 6144])
            wst = [SB(st, "wst%d" % i, [128, 8, 512]) for i in range(2)]
            op("sp", lambda e: e.dma_start(out=cc_t[:], in_=c_col), writes=["cc"], dma=True)
            op("sp", lambda e: e.dma_start(out=bada[:], in_=b_ada), writes=["bada"], dma=True)
            op("act", lambda e: e.activation(out=sc_t[:], in_=cc_t[:], func=AF.Silu), reads=["cc"], writes=["sc"])
            for cb in range(12):
                w = wst[cb % 2]
                wk = "wst%d" % (cb % 2)
                op("sp", lambda e, w=w, cb=cb: e.dma_start(
                    out=w[:], in_=w_ada[:, cb * 512:(cb + 1) * 512].rearrange("(kc p) n -> p kc n", p=128)),
                   writes=[wk], dma=True)
                for kc in range(8):
                    op("pe", lambda e, w=w, kc=kc: e.matmul(ps[0:1, 0, :], lhsT=sc_t[:, kc:kc + 1], rhs=w[:, kc, :],
                                                         start=(kc == 0), stop=(kc == 7)),
                       reads=[wk, "sc"], writes=["ps0"])
                op("dve", lambda e, cb=cb: e.tensor_tensor(out=modr[0:1, cb * 512:(cb + 1) * 512], in0=ps[0:1, 0, :],
                                                        in1=bada[0:1, cb * 512:(cb + 1) * 512], op=ALU.add),
                   reads=["ps0", "bada"], writes=["modr"])
            op("sp", lambda e: e.dma_start(out=modrow, in_=modr[:]), reads=["modr"], writes=["modrow"], dma=True)
        P.barrier()

        def mod_bc(dst, idx, key):
            op("sp", lambda e: e.dma_start(out=dst, in_=modrow[0:1, idx * 1024:(idx + 1) * 1024].partition_broadcast(128)),
               reads=["modrow"], writes=[key], dma=True)

        P.enabled = upto >= 1 and (only is None or only == 1)
        with contextlib.ExitStack() as st:
            g1 = SB(st, "g1", [128, 1024])
            shm = SB(st, "shm", [128, 1024])
            nm = SB(st, "nm", [128, 1024])
            bcast_load(nm[:], norm_mix, "nm")
            mod_bc(shm[:], 0, "shm")
            mod_bc(g1[:], 1, "g1")
            op("dve", lambda e: e.scalar_tensor_tensor(out=g1[:], in0=g1[:], scalar=1.0, in1=nm[:], op0=ALU.add, op1=ALU.mult),
               reads=["g1", "nm"], writes=["g1"])
            xt = [SB(st, "xt%d" % i, [128, 1024]) for i in range(2)]
            hb = [SB(st, "hb%d" % i, [128, 1024]) for i in range(2)]
            junk = SB(st, "junk", [128, 1024], BF16)
            hst = [SB(st, "hst%d" % i, [128, 8, 512], BF16) for i in range(2)]
            ssb = SB(st, "ssb", [128, 4])
            for stile in range(16):
                hs = hst[stile % 2]
                hk = "hst%d" % (stile % 2)
                for tt in range(4):
                    t = stile * 4 + tt
                    x_ = xt[t % 2]
                    xk = "xt%d" % (t % 2)
                    h_ = hb[t % 2]
                    hbk = "hb%d" % (t % 2)
                    op("sp", lambda e, x_=x_, t=t: e.dma_start(out=x_[:], in_=xl[t * 128:(t + 1) * 128, :]), writes=[xk], dma=True)
                    op("act", lambda e, x_=x_: e.activation(out=junk[:], in_=x_[:], func=AF.Square, accum_out=ssb[:, 0:1]),
                       reads=[xk], writes=["junk", "ssb"])
                    rstd_from_ss(ssb[:, 0:1], 1024, ssb[:, 1:2], ssb[:, 2:3], ["ssb"])
                    op("dve", lambda e, t=t: e.tensor_tensor(out=ssb[:, 1:2], in0=ssb[:, 1:2], in1=vtm[:, t:t + 1], op=ALU.mult),
                       reads=["ssb", "vtm"], writes=["ssb"])
                    op("dve", lambda e, x_=x_, h_=h_: e.scalar_tensor_tensor(out=h_[:], in0=x_[:], scalar=ssb[:, 1:2], in1=g1[:],
                                                                          op0=ALU.mult, op1=ALU.mult),
                       reads=[xk, "ssb", "g1"], writes=[hbk])
                    op("dve", lambda e, h_=h_, t=t: e.scalar_tensor_tensor(out=h_[:], in0=shm[:], scalar=vtm[:, t:t + 1], in1=h_[:],
                                                                           op0=ALU.mult, op1=ALU.add),
                       reads=[hbk, "shm", "vtm"], writes=[hbk])
                    b0 = (t % 2) * 2
                    for kc in range(8):
                        bk = b0 + kc // 4
                        op("pe", lambda e, h_=h_, kc=kc, bk=bk: e.transpose(out=ps[:, bk, (kc % 4) * 128:(kc % 4 + 1) * 128],
                                                                          in_=h_[:, kc * 128:(kc + 1) * 128], identity=ident[:]),
                           reads=[hbk, "ident"], writes=["ps%d" % bk])
                    op("act", lambda e, hs=hs, tt=tt, b0=b0: e.copy(
                        out=hs[:, :, tt * 128:(tt + 1) * 128],
                        in_=ps[:, b0:b0 + 2, :].rearrange("p a (b c) -> p (a b) c", c=128)),
                       reads=["ps%d" % b0, "ps%d" % (b0 + 1)], writes=[hk])
                op("sp", lambda e, hs=hs, stile=stile: e.dma_start(out=hT_d[:, :, stile * 512:(stile + 1) * 512], in_=hs[:]),
                   reads=[hk], writes=["hT_d"], dma=True)
            invf = SB(st, "invf", [128, 2])
            op("sp", lambda e: e.dma_start(out=invf[:], in_=invf_d), writes=["invf"], dma=True)
            pi_t = SB(st, "pi_t", [128, 512], I32)
            th = SB(st, "th", [128, 512])
            tf = SB(st, "tf", [128, 512])
            ti = SB(st, "ti", [128, 512], I32)
            rr = SB(st, "rr", [128, 512])
            for stile in range(16):
                sl = slice(stile * 512, (stile + 1) * 512)
                op("sp", lambda e, sl=sl: e.dma_start(out=pi_t[:], in_=posb[:, sl]), writes=["pi"], dma=True)
                for which in range(2):
                    op("dve", lambda e: e.tensor_copy(out=th[:], in_=pi_t[:]), reads=["pi"], writes=["th"])
                    op("dve", lambda e, which=which: e.tensor_scalar(out=th[:], in0=th[:], scalar1=invf[:, 0:1],
                                                                     scalar2=(0.0 if which == 0 else math.pi / 2),
                                                                     op0=ALU.mult, op1=ALU.add),
                       reads=["th", "invf"], writes=["th"])
                    op("dve", lambda e: e.tensor_scalar(out=tf[:], in0=th[:], scalar1=1.0 / TWO_PI, scalar2=None, op0=ALU.mult),
                       reads=["th"], writes=["tf"])
                    op("dve", lambda e: e.tensor_copy(out=ti[:], in_=tf[:]), reads=["tf"], writes=["ti"])
                    op("dve", lambda e: e.tensor_copy(out=tf[:], in_=ti[:]), reads=["ti"], writes=["tf"])
                    op("dve", lambda e: e.scalar_tensor_tensor(out=th[:], in0=tf[:], scalar=-TWO_PI, in1=th[:], op0=ALU.mult, op1=ALU.add),
                       reads=["tf", "th"], writes=["th"])
                    op("dve", lambda e: e.tensor_scalar(out=th[:], in0=th[:], scalar1=-math.pi, scalar2=math.pi, op0=ALU.max, op1=ALU.min),
                       reads=["th"], writes=["th"])
                    op("act", lambda e: e.activation(out=rr[:], in_=th[:], func=AF.Sin), reads=["th"], writes=["rr"])
                    if which == 0:
                        op("dve", lambda e: e.tensor_scalar(out=rr[:], in0=rr[:], scalar1=invf[:, 1:2], scalar2=None, op0=ALU.mult),
                           reads=["rr", "invf"], writes=["rr"])
                        op("sp", lambda e, sl=sl: e.dma_start(out=ropeS[:, sl], in_=rr[:]), reads=["rr"], writes=["ropeS"], dma=True)
                    else:
                        op("sp", lambda e, sl=sl: e.dma_start(out=ropeC[:, sl], in_=rr[:]), reads=["rr"], writes=["ropeC"], dma=True)
        P.barrier()

        P.enabled = upto >= 2 and (only is None or only == 2)
        with contextlib.ExitStack() as st:
            cmf = SB(st, "cmf", [128, 512])
            cmb = SB(st, "cmb", [128, 4, 512], BF16)
            for d in range(4):
                op("sp", lambda e, d=d: e.dma_start(out=cmf[:], in_=cmask_d[d]), writes=["cmf"], dma=True)
                op("dve", lambda e, d=d: e.tensor_copy(out=cmb[:, d, :], in_=cmf[:]), reads=["cmf"], writes=["cmb"])
            lv = SB(st, "lv", [128, 4, 64])
            for i in range(4):
                bcast_load(lv[:, i, :], lam_d[i:i + 1, :], "lv")
            lp = SB(st, "lp", [128, 2, 64])
            op("dve", lambda e: e.tensor_tensor(out=lp[:, 0, :], in0=lv[:, 0, :], in1=lv[:, 1, :], op=ALU.mult), reads=["lv"], writes=["lp"])
            op("dve", lambda e: e.tensor_tensor(out=lp[:, 1, :], in0=lv[:, 2, :], in1=lv[:, 3, :], op=ALU.mult), reads=["lv"], writes=["lp"])
            op("dve", lambda e: e.reduce_sum(out=small[:, 0:2], in_=lp[:], axis=AX.X), reads=["lp"], writes=["small"])
            op("act", lambda e: e.activation(out=small[:, 2:4], in_=small[:, 0:2], func=AF.Exp), reads=["small"], writes=["small"])
            op("dve", lambda e: e.scalar_tensor_tensor(out=small[:, 4:5], in0=small[:, 3:4], scalar=-0.2, in1=small[:, 2:3],
                                                       op0=ALU.add, op1=ALU.subtract), reads=["small"], writes=["small"])
            nlam = small[:, 4:5]
            hg = SB(st, "hg", [128, 128])
            bcast_load(hg[:], ahn, "hg")
            op("dve", lambda e: e.tensor_scalar(out=hg[:], in0=hg[:], scalar1=0.8, scalar2=None, op0=ALU.mult), reads=["hg"], writes=["hg"])

            wsf = [SB(st, "wsf%d" % i, [128, 8, 128]) for i in range(2)]
            W5 = [SB(st, "W5_%d" % i, [128, 5, 8, 128], BF16) for i in range(2)]
            hsb = [SB(st, "hsb%d" % i, [128, 8, 512], BF16) for i in range(2)]
            Ct = [SB(st, "Ct%d" % i, [128, 512]) for i in range(2)]
            St = [SB(st, "St%d" % i, [128, 512]) for i in range(2)]
            t1 = SB(st, "t1", [128, 512])
            t2 = SB(st, "t2", [128, 512])
            kT = SB(st, "kT", [128, 8192], BF16)
            qT = SB(st, "qT", [128, 2048], BF16)
            va = SB(st, "va", [128, 64, 130], BF16)
            E = [[SB(st, "E%d_%d" % (c, i), [128, 512], BF16) for i in range(2)] for c in range(2)]
            o0 = SB(st, "o0", [128, 128])
            o1 = SB(st, "o1", [128, 128])
            ojunk = SB(st, "ojunk", [128, 128])
            yst = SB(st, "yst", [128, 512], BF16)
            fs = SB(st, "fs", [128, 8])
            acc = [ps[:, 4 + 2 * c:6 + 2 * c, :].rearrange("p a (b c) -> p (a b) c", c=256) for c in range(2)]
            acck = [["ps4", "ps5"], ["ps6", "ps7"]]
            op("dve", lambda e: e.tensor_copy(out=va[:, :, 128], in_=vtm[:]), reads=["vtm"], writes=["va"])

            for h in range(8):
                Wb = W5[h % 2]
                Wk = "W5_%d" % (h % 2)
                for i in range(5):
                    wi = h * 5 + i
                    ws_ = wsf[wi % 2]
                    wsk = "wsf%d" % (wi % 2)
                    op("sp", lambda e, ws_=ws_, i=i, h=h: e.dma_start(out=ws_[:], in_=wqk[h, i].rearrange("(kc p) n -> p kc n", p=128)),
                       writes=[wsk], dma=True)
                    op("pool", lambda e, ws_=ws_, i=i, Wb=Wb: e.tensor_copy(out=Wb[:, i, :, :], in_=ws_[:]), reads=[wsk], writes=[Wk])
                for stile in range(16):
                    sl = slice(stile * 512, (stile + 1) * 512)
                    hs = hsb[stile % 2]
                    hk = "hsb%d" % (stile % 2)
                    C_ = Ct[stile % 2]
                    S_ = St[stile % 2]
                    ck = "Ct%d" % (stile % 2)
                    sk = "St%d" % (stile % 2)
                    op("sp", lambda e, hs=hs, sl=sl: e.dma_start(out=hs[:], in_=hT_d[:, :, sl]), reads=["hT_d"], writes=[hk], dma=True)
                    op("sp", lambda e, C_=C_, sl=sl: e.dma_start(out=C_[:], in_=ropeC[:, sl]), reads=["ropeC"], writes=[ck], dma=True)
                    op("sp", lambda e, S_=S_, sl=sl: e.dma_start(out=S_[:], in_=ropeS[:, sl]), reads=["ropeS"], writes=[sk], dma=True)
                    jobs = [(2, 3, kT, sl, "kT")]
                    if stile >= 12:
                        jobs.append((0, 1, qT, slice((stile - 12) * 512, (stile - 11) * 512), "qT"))
                    for (ia, ib, dst, dsl, dk) in jobs:
                        for kc in range(8):
                            op("pe", lambda e, kc=kc, ia=ia, hs=hs, Wb=Wb: e.matmul(ps[:, 0, :], lhsT=Wb[:, ia, kc, :], rhs=hs[:, kc, :],
                                                                                   start=(kc == 0), stop=(kc == 7)),
                               reads=[Wk, hk], writes=["ps0"])
                        for kc in range(8):
                            op("pe", lambda e, kc=kc, ib=ib, hs=hs, Wb=Wb: e.matmul(ps[:, 1, :], lhsT=Wb[:, ib, kc, :], rhs=hs[:, kc, :],
                                                                                   start=(kc == 0), stop=(kc == 7)),
                               reads=[Wk, hk], writes=["ps1"])
                        op("dve", lambda e, C_=C_: e.tensor_tensor(out=t1[:], in0=ps[:, 0, :], in1=C_[:], op=ALU.mult),
                           reads=["ps0", ck], writes=["t1"])
                        op("dve", lambda e, S_=S_: e.tensor_tensor(out=t2[:], in0=ps[:, 1, :], in1=S_[:], op=ALU.mult),
                           reads=["ps1", sk], writes=["t2"])
                        op("dve", lambda e, dst=dst, dsl=dsl: e.tensor_tensor(out=dst[:, dsl], in0=t1[:], in1=t2[:], op=ALU.add),
                           reads=["t1", "t2"], writes=[dk])
                    for tt in range(4):
                        for kc in range(8):
                            op("pe", lambda e, kc=kc, tt=tt, hs=hs, Wb=Wb: e.matmul(ps[:, 2, tt * 128:(tt + 1) * 128],
                                                                                   lhsT=hs[:, kc, tt * 128:(tt + 1) * 128], rhs=Wb[:, 4, kc, :],
                                                                                   start=(kc == 0), stop=(kc == 7)),
                               reads=[Wk, hk], writes=["ps2"])
                    for tt in range(4):
                        t = stile * 4 + tt
                        op("act", lambda e, tt=tt, t=t: e.activation(out=va[:, t, 0:128], in_=ps[:, 2, tt * 128:(tt + 1) * 128],
                                                                     func=AF.Copy, scale=vtm[:, t:t + 1]),
                           reads=["ps2", "vtm"], writes=["va"])
                for m in range(4):
                    kbase = 4 * (12 + m)
                    nkt = kbase + 4
                    qs_sl = slice(m * 512, (m + 1) * 512)
                    def emit_score(kt, kbase=kbase, qs_sl=qs_sl):
                        d = kt - kbase
                        for c in range(2):
                            pb = 2 * c + (kt % 2)
                            Eb = E[c][kt % 2]
                            ek = "E%d_%d" % (c, kt % 2)
                            op("pe", lambda e, c=c, kt=kt, pb=pb, qs_sl=qs_sl: e.matmul(
                                ps[:, pb, :], lhsT=kT[c * 64:(c + 1) * 64, kt * 128:(kt + 1) * 128],
                                rhs=qT[c * 64:(c + 1) * 64, qs_sl], start=True, stop=True),
                               reads=["kT", "qT"], writes=["ps%d" % pb])
                            op("act", lambda e, Eb=Eb, pb=pb: e.activation(out=Eb[:], in_=ps[:, pb, :], func=AF.Exp, scale=0.125),
                               reads=["ps%d" % pb], writes=[ek])
                            if d >= 0:
                                op("pool", lambda e, Eb=Eb, d=d: e.tensor_tensor(out=Eb[:], in0=Eb[:], in1=cmb[:, d, :], op=ALU.mult),
                                   reads=[ek, "cmb"], writes=[ek])

                    def emit_pv(kt, kbase=kbase):
                        d = kt - kbase
                        for c in range(2):
                            Eb = E[c][kt % 2]
                            ek = "E%d_%d" % (c, kt % 2)
                            for qs in range(4):
                                if d > qs:
                                    continue
                                op("pe", lambda e, c=c, qs=qs, kt=kt, Eb=Eb, kbase=kbase: e.matmul(
                                    acc[c][:, qs, 0:129], lhsT=Eb[:, qs * 128:(qs + 1) * 128], rhs=va[:, kt, 0:129],
                                    start=(kt == 0), stop=(kt == kbase + qs)),
                                   reads=[ek, "va"], writes=[acck[c][qs // 2]])

                    for kt in range(nkt + 1):
                        if kt < nkt:
                            emit_score(kt)
                        if kt >= 1:
                            emit_pv(kt - 1)
                    for qs in range(4):
                        a0 = acc[0][:, qs, :]
                        a1 = acc[1][:, qs, :]
                        k0 = acck[0][qs // 2]
                        k1 = acck[1][qs // 2]
                        op("dve", lambda e, a0=a0: e.reciprocal(out=fs[:, 0:1], in_=a0[:, 128:129]), reads=[k0], writes=["fs"])
                        op("dve", lambda e, a1=a1: e.reciprocal(out=fs[:, 1:2], in_=a1[:, 128:129]), reads=[k1], writes=["fs"])
                        op("dve", lambda e: e.tensor_scalar(out=fs[:, 2:3], in0=fs[:, 1:2], scalar1=nlam, scalar2=None, op0=ALU.mult),
                           reads=["fs", "small"], writes=["fs"])
                        op("dve", lambda e, a0=a0: e.tensor_scalar(out=o0[:], in0=a0[:, 0:128], scalar1=fs[:, 0:1], scalar2=None, op0=ALU.mult),
                           reads=[k0, "fs"], writes=["o0"])
                        op("dve", lambda e, a1=a1: e.scalar_tensor_tensor(out=o1[:], in0=a1[:, 0:128], scalar=fs[:, 2:3], in1=o0[:],
                                                                          op0=ALU.mult, op1=ALU.add),
                           reads=[k1, "fs", "o0"], writes=["o1"])
                        op("act", lambda e: e.activation(out=ojunk[:], in_=o1[:], func=AF.Square, accum_out=fs[:, 3:4]),
                           reads=["o1"], writes=["ojunk", "fs"])
                        rstd_from_ss(fs[:, 3:4], 128, fs[:, 4:5], fs[:, 5:6], ["fs"])
                        op("dve", lambda e: e.scalar_tensor_tensor(out=o0[:], in0=o1[:], scalar=fs[:, 4:5], in1=hg[:], op0=ALU.mult, op1=ALU.mult),
                           reads=["o1", "fs", "hg"], writes=["o0"])
                        op("pe", lambda e, qs=qs: e.transpose(out=ps[:, 0, qs * 128:(qs + 1) * 128], in_=o0[:], identity=ident[:]),
                           reads=["o0", "ident"], writes=["ps0"])
                    op("act", lambda e: e.copy(out=yst[:], in_=ps[:, 0, :]), reads=["ps0"], writes=["yst"])
                    op("sp", lambda e, h=h, qs_sl=qs_sl: e.dma_start(out=yaT_d[:, h, qs_sl], in_=yst[:]), reads=["yst"], writes=["yaT_d"], dma=True)
        P.barrier()

        P.enabled = upto >= 3 and (only is None or only == 3)
        with contextlib.ExitStack() as st:
            wx = SB(st, "wx", [128, 8, 3072], BF16)
            wdt = SB(st, "wdt", [128, 8, 32], BF16)
            with contextlib.ExitStack() as st2:
                wst3 = [SB(st2, "wst3_%d" % i, [128, 8, 512]) for i in range(2)]
                for cb in range(6):
                    w = wst3[cb % 2]
                    wk = "wst3_%d" % (cb % 2)
                    op("sp", lambda e, w=w, cb=cb: e.dma_start(out=w[:], in_=wxbc_d[:, cb * 512:(cb + 1) * 512].rearrange("(kc p) n -> p kc n", p=128)),
                       writes=[wk], dma=True)
                    op("pool" if cb % 2 else "dve", lambda e, w=w, cb=cb: e.tensor_copy(out=wx[:, :, cb * 512:(cb + 1) * 512], in_=w[:]),
                       reads=[wk], writes=["wx"])
                w = wst3[0]
                op("sp", lambda e, w=w: e.dma_start(out=w[:, :, 0:32], in_=wdt_d.rearrange("(kc p) n -> p kc n", p=128)), writes=["wst3_0"], dma=True)
                op("dve", lambda e, w=w: e.tensor_copy(out=wdt[:], in_=w[:, :, 0:32]), reads=["wst3_0"], writes=["wdt"])
                P.barrier()
            cw = SB(st, "cw", [128, 24, 4])
            cbias = SB(st, "cbias", [128, 24])
            op("sp", lambda e: e.dma_start(out=cw[:], in_=convw_d), writes=["cw"], dma=True)
            op("sp", lambda e: e.dma_start(out=cbias[:], in_=convb_d), writes=["cbias"], dma=True)
            dtb = SB(st, "dtb", [128, 32])
            aneg = SB(st, "aneg", [128, 32])
            dsk = SB(st, "dsk", [128, 32])
            bcast_load(dtb[:], dtb_d, "dtb")
            bcast_load(aneg[:], alog_d, "aneg")
            bcast_load(dsk[:], dskip_d, "dsk")
            op("act", lambda e: e.activation(out=aneg[:], in_=aneg[:], func=AF.Exp), reads=["aneg"], writes=["aneg"])
            op("dve", lambda e: e.tensor_scalar(out=aneg[:], in0=aneg[:], scalar1=-1.0, scalar2=None, op0=ALU.mult), reads=["aneg"], writes=["aneg"])
            hsb = [SB(st, "h3_%d" % i, [128, 8, 512], BF16) for i in range(2)]
            vbt = [SB(st, "vb%d" % i, [128, 512]) for i in range(2)]
            pre = [SB(st, "pre%d" % i, [128, 515]) for i in range(2)]
            cv = [SB(st, "cv%d" % i, [128, 512]) for i in range(2)]
            slb = [SB(st, "sl%d" % i, [128, 512]) for i in range(2)]
            halo = SB(st, "halo", [128, 24, 3])
            x_tm = SB(st, "x_tm", [128, 4, 2048])
            xdt = SB(st, "xdt", [128, 4, 2048], BF16)
            xdw = SB(st, "xdw", [128, 4, 2048], BF16)
            xtmp = SB(st, "xtmp", [128, 2048])
            B_tm = SB(st, "B_tm", [128, 4, 512], BF16)
            B_cm = SB(st, "B_cm", [128, 4, 512], BF16)
            C_cm = SB(st, "C_cm", [128, 4, 512], BF16)
            ST = SB(st, "ST", [128, 2048])
            STb = SB(st, "STb", [128, 2048], BF16)
            dtr = SB(st, "dtr", [128, 4, 32])
            a_tm = SB(st, "a_tm", [128, 4, 32])
            acs4 = SB(st, "acs4", [128, 4, 64])
            wend4 = SB(st, "wend4", [128, 4, 32])
            etot4 = SB(st, "etot4", [128, 4, 32])
            eac4 = SB(st, "eac4", [128, 4, 32])
            dw4 = SB(st, "dw4", [128, 4, 32])
            cbm = SB(st, "cbm", [128, 128])
            arg = SB(st, "arg", [128, 128])
            dec = SB(st, "dec", [128, 128])
            arg8 = SB(st, "arg8", [128, 8, 128])
            dec8 = SB(st, "dec8", [128, 8, 128])
            MT8 = SB(st, "MT8", [128, 8, 128], BF16)
            ypre = SB(st, "ypre", [128, 2048])
            op("dve", lambda e: e.memset(halo[:], 0.0), writes=["halo"])
            op("dve", lambda e: e.memset(ST[:], 0.0), writes=["ST"])
            op("dve", lambda e: e.memset(STb[:], 0.0), writes=["STb"])

            def bc_h(ap32, nrep=64):
                return ap32.unsqueeze(2).to_broadcast([128, 32, nrep])

            cci = 0
            for stile in range(16):
                sl = slice(stile * 512, (stile + 1) * 512)
                hs = hsb[stile % 2]
                hk = "h3_%d" % (stile % 2)
                vb = vbt[stile % 2]
                vk = "vb%d" % (stile % 2)
                op("sp", lambda e, hs=hs, sl=sl: e.dma_start(out=hs[:], in_=hT_d[:, :, sl]), reads=["hT_d"], writes=[hk], dma=True)
                for cc in range(24):
                    pb = cci % 2
                    cci += 1
                    pr = pre[pb]
                    prk = "pre%d" % pb
                    cv_ = cv[pb]
                    cvk = "cv%d" % pb
                    sl_ = slb[pb]
                    slk = "sl%d" % pb
                    for kc in range(8):
                        op("pe", lambda e, kc=kc, cc=cc, hs=hs, pb=pb: e.matmul(ps[:, pb, :], lhsT=wx[:, kc, cc * 128:(cc + 1) * 128], rhs=hs[:, kc, :],
                                                                               start=(kc == 0), stop=(kc == 7)),
                           reads=["wx", hk], writes=["ps%d" % pb])
                    op("act", lambda e, pr=pr, pb=pb: e.copy(out=pr[:, 3:515], in_=ps[:, pb, :]),
                       reads=["ps%d" % pb], writes=[prk])
                    op("pool", lambda e, pr=pr, cc=cc: e.tensor_copy(out=pr[:, 0:3], in_=halo[:, cc, :]), reads=[("halo", cc)], writes=[prk])
                    op("pool", lambda e, pr=pr, cc=cc: e.tensor_copy(out=halo[:, cc, :], in_=pr[:, 512:515]), reads=[prk], writes=[("halo", cc)])
                    op("dve", lambda e, pr=pr, cv_=cv_, cc=cc: e.tensor_scalar(out=cv_[:], in0=pr[:, 0:512], scalar1=cw[:, cc, 0:1],
                                                                               scalar2=cbias[:, cc:cc + 1], op0=ALU.mult, op1=ALU.add),
                       reads=[prk, "cw", "cbias"], writes=[cvk])
                    for k in range(1, 4):
                        op("dve", lambda e, pr=pr, cv_=cv_, cc=cc, k=k: e.scalar_tensor_tensor(out=cv_[:], in0=pr[:, k:k + 512], scalar=cw[:, cc, k:k + 1],
                                                                                               in1=cv_[:], op0=ALU.mult, op1=ALU.add),
                           reads=[prk, "cw"], writes=[cvk])
                    if cc < 20:
                        op("act", lambda e, cv_=cv_, sl_=sl_: e.activation(out=sl_[:], in_=cv_[:], func=AF.Silu), reads=[cvk], writes=[slk])
                        for tt in range(4):
                            op("pe", lambda e, tt=tt, sl_=sl_: e.transpose(out=ps[:, 2, tt * 128:(tt + 1) * 128], in_=sl_[:, tt * 128:(tt + 1) * 128],
                                                                          identity=ident[:]),
                               reads=[slk, "ident"], writes=["ps2"])
                        src = ps[:, 2, :].rearrange("p (a b) -> p a b", b=128)
                        if cc < 16:
                            op("act", lambda e, cc=cc, src=src: e.copy(out=x_tm[:, :, cc * 128:(cc + 1) * 128], in_=src),
                               reads=["ps2"], writes=["x_tm"])
                        else:
                            g = cc - 16
                            op("act", lambda e, g=g, src=src: e.copy(out=B_tm[:, :, g * 128:(g + 1) * 128], in_=src),
                               reads=["ps2"], writes=["B_tm"])
                            op("pool", lambda e, g=g, sl_=sl_: e.tensor_copy(out=B_cm[:, g, :], in_=sl_[:]), reads=[slk], writes=["B_cm"])
                    else:
                        g = cc - 20
                        op("act", lambda e, cv_=cv_, g=g: e.activation(out=C_cm[:, g, :], in_=cv_[:], func=AF.Silu), reads=[cvk], writes=["C_cm"])
                if upto == 3 and SUB < 2:
                    continue
                for tt in range(4):
                    for kc in range(8):
                        op("pe", lambda e, kc=kc, tt=tt, hs=hs: e.matmul(ps[:, 3, tt * 32:(tt + 1) * 32], lhsT=hs[:, kc, tt * 128:(tt + 1) * 128],
                                                                        rhs=wdt[:, kc, :], start=(kc == 0), stop=(kc == 7)),
                           reads=["wdt", hk], writes=["ps3"])
                op("dve", lambda e: e.tensor_tensor(out=dtr[:], in0=ps[:, 3, 0:128].rearrange("p (a b) -> p a b", b=32),
                                                    in1=dtb[:].unsqueeze(1).to_broadcast([128, 4, 32]), op=ALU.add),
                   reads=["ps3", "dtb"], writes=["dtr"])
                op("act", lambda e: e.activation(out=dtr[:], in_=dtr[:], func=AF.Exp), reads=["dtr"], writes=["dtr"])
                op("act", lambda e: e.activation(out=dtr[:], in_=dtr[:], func=AF.Ln, bias=1.0), reads=["dtr"], writes=["dtr"])
                for tt in range(4):
                    t = stile * 4 + tt
                    op("dve", lambda e, tt=tt, t=t: e.tensor_scalar(out=dtr[:, tt, :], in0=dtr[:, tt, :], scalar1=vtm[:, t:t + 1], scalar2=None, op0=ALU.mult),
                       reads=["dtr", "vtm"], writes=["dtr"])
                op("dve", lambda e: e.tensor_tensor(out=a_tm[:], in0=dtr[:], in1=aneg[:].unsqueeze(1).to_broadcast([128, 4, 32]), op=ALU.mult),
                   reads=["dtr", "aneg"], writes=["a_tm"])
                own = stile >= 12 and not (upto == 3 and SUB < 4)
                HL = not (upto == 3 and SUB < 5)
                if upto == 3 and SUB < 3:
                    continue
                for tt in range(4):
                    c0 = 128 + tt * 64
                    op("pe", lambda e, tt=tt, c0=c0: e.matmul(ps[:, 3, c0:c0 + 32], lhsT=tri[:], rhs=a_tm[:, tt, :], start=True, stop=True),
                       reads=["tri", "a_tm"], writes=["ps3"])
                    op("pe", lambda e, tt=tt, c0=c0: e.matmul(ps[:, 3, c0 + 32:c0 + 64], lhsT=ones[:], rhs=a_tm[:, tt, :], start=True, stop=True),
                       reads=["ones", "a_tm"], writes=["ps3"])
                op("dve", lambda e: e.tensor_copy(out=acs4[:], in_=ps[:, 3, 128:384].rearrange("p (a b) -> p a b", b=64)), reads=["ps3"], writes=["acs"])
                op("dve", lambda e: e.tensor_tensor(out=wend4[:], in0=acs4[:, :, 32:64], in1=acs4[:, :, 0:32], op=ALU.subtract), reads=["acs"], writes=["wend"])
                op("act", lambda e: e.activation(out=wend4[:], in_=wend4[:], func=AF.Exp), reads=["wend"], writes=["wend"])
                op("act", lambda e: e.activation(out=etot4[:], in_=acs4[:, :, 32:64], func=AF.Exp), reads=["acs"], writes=["etot"])
                if own:
                    op("act", lambda e: e.activation(out=eac4[:], in_=acs4[:, :, 0:32], func=AF.Exp), reads=["acs"], writes=["eac"])
                op("dve", lambda e: e.tensor_tensor(out=dw4[:], in0=dtr[:], in1=wend4[:], op=ALU.mult), reads=["dtr", "wend"], writes=["dw4"])
                for tt in range(4):
                    op("dve", lambda e, tt=tt: e.tensor_tensor(out=xdt[:, tt, :].rearrange("p (h d) -> p h d", d=64),
                                                               in0=x_tm[:, tt, :].rearrange("p (h d) -> p h d", d=64),
                                                               in1=bc_h(dtr[:, tt, :]), op=ALU.mult),
                       reads=["x_tm", "dtr"], writes=[("xdt", tt)])
                    op("dve", lambda e, tt=tt: e.tensor_tensor(out=xdw[:, tt, :].rearrange("p (h d) -> p h d", d=64),
                                                               in0=x_tm[:, tt, :].rearrange("p (h d) -> p h d", d=64),
                                                               in1=bc_h(dw4[:, tt, :]), op=ALU.mult),
                       reads=["x_tm", "dw4"], writes=[("xdw", tt)])
                for tt in range(4):
                    if own:
                        tsl = slice(tt * 128, (tt + 1) * 128)
                        for g in range(4):
                            op("pe", lambda e, g=g, tsl=tsl: e.matmul(ps[:, 7, 0:128], lhsT=B_cm[:, g, tsl], rhs=C_cm[:, g, tsl], start=True, stop=True),
                               reads=["B_cm", "C_cm"], writes=["ps7"])
                            op("dve", lambda e: e.tensor_tensor(out=cbm[:], in0=ps[:, 7, 0:128], in1=tri[:], op=ALU.mult),
                               reads=["ps7", "tri"], writes=["cbm"])
                            op("pe", lambda e, g=g, tsl=tsl: e.matmul(ps[:, 4, :], lhsT=C_cm[:, g, tsl], rhs=STb[:, g * 512:(g + 1) * 512], start=True, stop=True),
                               reads=["C_cm", "STb"], writes=["ps4"])
                            for hh in range(8):
                                hd = g * 8 + hh
                                rb_ = 6 + hh // 4
                                op("pe", lambda e, tt=tt, hd=hd, hh=hh, rb_=rb_: e.matmul(ps[:, rb_, (hh % 4) * 128:(hh % 4 + 1) * 128],
                                                                                      lhsT=a_tm[:, tt, hd:hd + 1].to_broadcast([128, 128]),
                                                                                      rhs=tri[:], start=True, stop=True),
                                   reads=["a_tm", "tri"], writes=["ps%d" % rb_])
                            for hh in range(8):
                                hd = g * 8 + hh
                                rb_ = 6 + hh // 4
                                op("dve", lambda e, hd=hd, hh=hh, rb_=rb_, tt=tt: e.tensor_scalar(out=arg8[:, hh, :], in0=ps[:, rb_, (hh % 4) * 128:(hh % 4 + 1) * 128],
                                                                                       scalar1=acs4[:, tt, hd:hd + 1], scalar2=0.0, op0=ALU.subtract, op1=ALU.min),
                                   reads=["ps%d" % rb_, "acs"], writes=["arg8"])
                            op("act", lambda e: e.activation(out=dec8[:], in_=arg8[:], func=AF.Exp), reads=["arg8"], writes=["dec8"])
                            op("dve", lambda e: e.tensor_tensor(out=MT8[:], in0=dec8[:], in1=cbm[:].unsqueeze(1).to_broadcast([128, 8, 128]), op=ALU.mult),
                               reads=["dec8", "cbm"], writes=["MT8"])
                            for hh in range(8):
                                hd = g * 8 + hh
                                op("pe", lambda e, hh=hh, hd=hd, tt=tt: e.matmul(ps[:, 5, hh * 64:(hh + 1) * 64], lhsT=MT8[:, hh, :],
                                                                               rhs=xdt[:, tt, hd * 64:(hd + 1) * 64], start=True, stop=True),
                                   reads=["MT8", ("xdt", tt)], writes=["ps5"])
                            gsl = slice(g * 512, (g + 1) * 512)
                            op("dve", lambda e, g=g, gsl=gsl, tt=tt: e.tensor_tensor(out=ypre[:, gsl].rearrange("p (h d) -> p h d", d=64),
                                                                              in0=ps[:, 4, :].rearrange("p (h d) -> p h d", d=64),
                                                                              in1=eac4[:, tt, g * 8:(g + 1) * 8].unsqueeze(2).to_broadcast([128, 8, 64]), op=ALU.mult),
                               reads=["ps4", "eac"], writes=["ypre"])
                            if HL:
                                op("dve", lambda e, gsl=gsl: e.tensor_tensor(out=ypre[:, gsl], in0=ypre[:, gsl], in1=ps[:, 5, :], op=ALU.add),
                                   reads=["ps5", "ypre"], writes=["ypre"])
                        op("dve", lambda e, tt=tt: e.tensor_tensor(out=xtmp[:].rearrange("p (h d) -> p h d", d=64),
                                                                   in0=x_tm[:, tt, :].rearrange("p (h d) -> p h d", d=64),
                                                                   in1=bc_h(dsk[:]), op=ALU.mult),
                           reads=["x_tm", "dsk", ("xdw", tt), ("xdt", tt)], writes=["xtmp"])
                        op("dve", lambda e: e.tensor_tensor(out=ypre[:], in0=ypre[:], in1=xtmp[:], op=ALU.add), reads=["xtmp", "ypre"], writes=["ypre"])
                        r0 = (stile - 12) * 512 + tt * 128
                        op("sp", lambda e, r0=r0: e.dma_start(out=ypre_d[r0:r0 + 128, :], in_=ypre[:]), reads=["ypre"], writes=["ypre_d"], dma=True)
                    for g in range(4):
                        pbk = 4 + g
                        gsl = slice(g * 512, (g + 1) * 512)
                        op("pe", lambda e, g=g, tt=tt, pbk=pbk, gsl=gsl: e.matmul(ps[:, pbk, :], lhsT=B_tm[:, tt, g * 128:(g + 1) * 128], rhs=xdw[:, tt, gsl],
                                                                                 start=True, stop=True),
                           reads=["B_tm", ("xdw", tt)], writes=["ps%d" % pbk])
                        op("dve", lambda e, g=g, gsl=gsl, tt=tt: e.tensor_tensor(out=ST[:, gsl].rearrange("p (h d) -> p h d", d=64),
                                                                          in0=ST[:, gsl].rearrange("p (h d) -> p h d", d=64),
                                                                          in1=etot4[:, tt, g * 8:(g + 1) * 8].unsqueeze(2).to_broadcast([128, 8, 64]), op=ALU.mult),
                           reads=["ST", "etot"], writes=["ST"])
                        op("dve", lambda e, pbk=pbk, gsl=gsl: e.tensor_tensor(out=ST[:, gsl], in0=ST[:, gsl], in1=ps[:, pbk, :], op=ALU.add),
                           reads=["ST", "ps%d" % pbk], writes=["ST"])
                    if stile >= 11:
                        op("act", lambda e: e.copy(out=STb[:], in_=ST[:]), reads=["ST"], writes=["STb"])
        P.barrier()

        P.enabled = upto >= 4 and (only is None or only == 4)
        def load_w_bf16(st, name, src, kchunks, ncols, engs=("dve", "pool")):
            wt = SB(st, name, [128, kchunks, ncols], BF16)
            with contextlib.ExitStack() as st2:
                stg = [SB(st2, name + "_s%d" % i, [128, 8, 512]) for i in range(2)]
                i = 0
                for k0 in range(0, kchunks, 8):
                    for cb in range(ncols // 512):
                        s_ = stg[i % 2]
                        sk_ = name + "_s%d" % (i % 2)
                        op("sp", lambda e, s_=s_, k0=k0, cb=cb: e.dma_start(
                            out=s_[:], in_=src[k0 * 128:(k0 + 8) * 128, cb * 512:(cb + 1) * 512].rearrange("(kc p) n -> p kc n", p=128)),
                           writes=[sk_], dma=True)
                        op(engs[i % 2], lambda e, s_=s_, k0=k0, cb=cb: e.tensor_copy(out=wt[:, k0:k0 + 8, cb * 512:(cb + 1) * 512], in_=s_[:]),
                           reads=[sk_], writes=[name])
                        i += 1
                P.barrier()
            return wt

        with contextlib.ExitStack() as st:
            wz = load_w_bf16(st, "wz", wz_d, 8, 2048)
            wbs = load_w_bf16(st, "wbs", wbs_d, 16, 1024)
            ssn = SB(st, "ssn", [128, 2048])
            bcast_load(ssn[:], ssdn_d, "ssn")
            hto = [SB(st, "hto%d" % i, [128, 8, 128], BF16) for i in range(2)]
            ypt = [SB(st, "ypt%d" % i, [128, 2048]) for i in range(2)]
            zs = SB(st, "zs", [128, 512])
            yg = SB(st, "yg", [128, 512])
            yjunk = SB(st, "yjunk", [128, 512], BF16)
            ysT = SB(st, "ysT", [128, 16, 128], BF16)
            psb = SB(st, "psb", [128, 1024])
            f4 = SB(st, "f4", [128, 4])
            for t in range(16):
                ht = hto[t % 2]
                htk = "hto%d" % (t % 2)
                yp = ypt[t % 2]
                ypk = "ypt%d" % (t % 2)
                c0 = 6144 + t * 128
                op("sp", lambda e, ht=ht, c0=c0: e.dma_start(out=ht[:], in_=hT_d[:, :, c0:c0 + 128]), reads=["hT_d"], writes=[htk], dma=True)
                op("sp", lambda e, yp=yp, t=t: e.dma_start(out=yp[:], in_=ypre_d[t * 128:(t + 1) * 128, :]), reads=["ypre_d"], writes=[ypk], dma=True)
                for cb in range(4):
                    csl = slice(cb * 512, (cb + 1) * 512)
                    pb = cb % 2
                    for kc in range(8):
                        op("pe", lambda e, kc=kc, ht=ht, csl=csl, pb=pb: e.matmul(ps[:, pb, :], lhsT=ht[:, kc, :], rhs=wz[:, kc, csl],
                                                                                 start=(kc == 0), stop=(kc == 7)),
                           reads=[htk, "wz"], writes=["ps%d" % pb])
                    op("act", lambda e, pb=pb: e.activation(out=zs[:], in_=ps[:, pb, :], func=AF.Silu), reads=["ps%d" % pb], writes=["zs"])
                    op("dve", lambda e, yp=yp, csl=csl: e.tensor_tensor(out=yg[:], in0=yp[:, csl], in1=zs[:], op=ALU.mult), reads=[ypk, "zs"], writes=["yg"])
                    op("act", lambda e: e.activation(out=yjunk[:], in_=yg[:], func=AF.Square, accum_out=f4[:, 0:1]), reads=["yg"], writes=["yjunk", "f4"])
                    rstd_from_ss(f4[:, 0:1], 512, f4[:, 1:2], f4[:, 2:3], ["f4"])
                    op("dve", lambda e, csl=csl: e.scalar_tensor_tensor(out=yg[:], in0=yg[:], scalar=f4[:, 1:2], in1=ssn[:, csl], op0=ALU.mult, op1=ALU.mult),
                       reads=["yg", "f4", "ssn"], writes=["yg"])
                    for i in range(4):
                        op("pe", lambda e, i=i: e.transpose(out=ps[:, 2, i * 128:(i + 1) * 128], in_=yg[:, i * 128:(i + 1) * 128], identity=ident[:]),
                           reads=["yg", "ident"], writes=["ps2"])
                    op("act", lambda e, cb=cb: e.copy(out=ysT[:, cb * 4:(cb + 1) * 4, :], in_=ps[:, 2, :].rearrange("p (a b) -> p a b", b=128)),
                       reads=["ps2"], writes=["ysT"])
                for half in range(2):
                    for kc in range(16):
                        op("pe", lambda e, kc=kc, half=half: e.matmul(ps[:, 4 + half, :], lhsT=ysT[:, kc, :], rhs=wbs[:, kc, half * 512:(half + 1) * 512],
                                                                     start=(kc == 0), stop=(kc == 15)),
                           reads=["ysT", "wbs"], writes=["ps%d" % (4 + half)])
                op("dve", lambda e: e.tensor_copy(out=psb[:].rearrange("p (a b) -> p a b", b=512), in_=ps[:, 4:6, :]), reads=["ps4", "ps5"], writes=["psb"])
                op("sp", lambda e, t=t: e.dma_start(out=ps_d[t * 128:(t + 1) * 128, :], in_=psb[:]), reads=["psb"], writes=["ps_d"], dma=True)
        P.barrier()

        P.enabled = upto >= 5 and (only is None or only == 5)
        with contextlib.ExitStack() as stB:
            h2T = SB(stB, "h2T", [128, 8, 2048], BF16)
            G = SB(stB, "G", [128, 16, 257])
            for t_ in range(16):
                op("dve", lambda e, t_=t_: e.memset(G[:, t_, :], 1.0), writes=["G"])
            with contextlib.ExitStack() as st:
                wg = load_w_bf16(st, "wg", wg_d, 8, 2048)
                wba = load_w_bf16(st, "wba", wba_d, 8, 1024)
                wo = load_w_bf16(st, "wo", wo_d, 8, 1024)
                wr = SB(st, "wr", [128, 8, 256])
                op("sp", lambda e: e.dma_start(out=wr[:], in_=wr_d.rearrange("(kc p) n -> p kc n", p=128)), writes=["wr"], dma=True)
                bg = SB(st, "bg", [128, 2048])
                bcast_load(bg[:], b_gate, "bg")
                gm_ = SB(st, "gm_", [128, 1024])
                mod_bc(gm_[:], 2, "gm_")
                shf = SB(st, "shf", [128, 1024])
                mod_bc(shf[:], 3, "shf")
                g2 = SB(st, "g2", [128, 1024])
                mod_bc(g2[:], 4, "g2")
                h2 = SB(st, "h2", [128, 1024])
                bcast_load(h2[:], norm_ffn, "h2")
                op("dve", lambda e: e.scalar_tensor_tensor(out=g2[:], in0=g2[:], scalar=1.0, in1=h2[:], op0=ALU.add, op1=ALU.mult),
                   reads=["g2", "h2"], writes=["g2"])
                rb = SB(st, "rb", [128, 256])
                bcast_load(rb[:], rbias_d, "rb")
                hto = [SB(st, "htb%d" % i, [128, 8, 128], BF16) for i in range(2)]
                yat = [SB(st, "yat%d" % i, [128, 8, 128], BF16) for i in range(2)]
                pst = [SB(st, "pst%d" % i, [128, 1024]) for i in range(2)]
                xo = [SB(st, "xo%d" % i, [128, 1024]) for i in range(2)]
                gt = SB(st, "gt", [128, 2048])
                mixed = SB(st, "mixed", [128, 1024])
                mT = SB(st, "mT", [128, 8, 128], BF16)
                x1 = SB(st, "x1", [128, 1024])
                hjunk = SB(st, "hjunk", [128, 1024], BF16)
                h2Tf = SB(st, "h2Tf", [128, 8, 128])
                scr = SB(st, "scr", [128, 256])
                cho = SB(st, "cho", [128, 256])
                msk = SB(st, "msk", [128, 256])
                m8 = SB(st, "m8", [128, 8, 8])
                gs = SB(st, "gs", [128, 8])
                t8 = SB(st, "t8", [128, 8])
                gmk = SB(st, "gmk", [128, 8])
                pen = SB(st, "pen", [128, 8])
                m8b = SB(st, "m8b", [128, 8])
                f5 = SB(st, "f5", [128, 8])
                for t in range(16):
                    ht = hto[t % 2]
                    htk = "htb%d" % (t % 2)
                    ya = yat[t % 2]
                    yak = "yat%d" % (t % 2)
                    pt = pst[t % 2]
                    ptk = "pst%d" % (t % 2)
                    xo_ = xo[t % 2]
                    xok = "xo%d" % (t % 2)
                    c0 = 6144 + t * 128
                    tsl = slice(t * 128, (t + 1) * 128)
                    op("sp", lambda e, ht=ht, c0=c0: e.dma_start(out=ht[:], in_=hT_d[:, :, c0:c0 + 128]), reads=["hT_d"], writes=[htk], dma=True)
                    op("sp", lambda e, ya=ya, tsl=tsl: e.dma_start(out=ya[:], in_=yaT_d[:, :, tsl]), reads=["yaT_d"], writes=[yak], dma=True)
                    op("sp", lambda e, pt=pt, tsl=tsl: e.dma_start(out=pt[:], in_=ps_d[tsl, :]), reads=["ps_d"], writes=[ptk], dma=True)
                    op("sp", lambda e, xo_=xo_, c0=c0: e.dma_start(out=xo_[:], in_=xl[c0:c0 + 128, :]), writes=[xok], dma=True)
                    for cb in range(4):
                        csl = slice(cb * 512, (cb + 1) * 512)
                        pb = cb % 2
                        for kc in range(8):
                            op("pe", lambda e, kc=kc, ht=ht, csl=csl, pb=pb: e.matmul(ps[:, pb, :], lhsT=ht[:, kc, :], rhs=wg[:, kc, csl],
                                                                                     start=(kc == 0), stop=(kc == 7)),
                               reads=[htk, "wg"], writes=["ps%d" % pb])
                        op("dve", lambda e, csl=csl, pb=pb: e.tensor_tensor(out=gt[:, csl], in0=ps[:, pb, :], in1=bg[:, csl], op=ALU.add),
                           reads=["ps%d" % pb, "bg"], writes=["gt"])
                    op("act", lambda e: e.activation(out=gt[:], in_=gt[:], func=AF.Sigmoid), reads=["gt"], writes=["gt"])
                    if upto == 5 and SUB < 3:
                        continue
                    for half in range(2):
                        hsl = slice(half * 512, (half + 1) * 512)
                        for kc in range(8):
                            op("pe", lambda e, kc=kc, ya=ya, hsl=hsl, half=half: e.matmul(ps[:, 2 + half, :], lhsT=ya[:, kc, :], rhs=wba[:, kc, hsl],
                                                                                         start=(kc == 0), stop=(kc == 7)),
                               reads=[yak, "wba"], writes=["ps%d" % (2 + half)])
                        op("dve", lambda e, hsl=hsl, half=half: e.tensor_tensor(out=mixed[:, hsl], in0=ps[:, 2 + half, :], in1=gt[:, hsl], op=ALU.mult),
                           reads=["ps%d" % (2 + half), "gt"], writes=["mixed"])
                    op("dve", lambda e, pt=pt: e.tensor_tensor(out=h2[:], in0=pt[:], in1=gt[:, 1024:2048], op=ALU.mult), reads=[ptk, "gt"], writes=["h2"])
                    op("dve", lambda e: e.tensor_tensor(out=mixed[:], in0=mixed[:], in1=h2[:], op=ALU.add), reads=["mixed", "h2"], writes=["mixed"])
                    if upto == 5 and SUB < 4:
                        continue
                    for kc in range(8):
                        bk = 4 + kc // 4
                        op("pe", lambda e, kc=kc, bk=bk: e.transpose(out=ps[:, bk, (kc % 4) * 128:(kc % 4 + 1) * 128], in_=mixed[:, kc * 128:(kc + 1) * 128],
                                                                    identity=ident[:]),
                           reads=["mixed", "ident"], writes=["ps%d" % bk])
                    op("act", lambda e: e.copy(out=mT[:], in_=ps[:, 4:6, :].rearrange("p a (b c) -> p (a b) c", c=128)), reads=["ps4", "ps5"], writes=["mT"])
                    for half in range(2):
                        hsl = slice(half * 512, (half + 1) * 512)
                        for kc in range(8):
                            op("pe", lambda e, kc=kc, hsl=hsl, half=half: e.matmul(ps[:, 6 + half, :], lhsT=mT[:, kc, :], rhs=wo[:, kc, hsl],
                                                                                  start=(kc == 0), stop=(kc == 7)),
                               reads=["mT", "wo"], writes=["ps%d" % (6 + half)])
                        op("dve", lambda e, hsl=hsl, half=half: e.tensor_tensor(out=x1[:, hsl], in0=ps[:, 6 + half, :], in1=gm_[:, hsl], op=ALU.mult),
                           reads=["ps%d" % (6 + half), "gm_"], writes=["x1"])
                    op("dve", lambda e, xo_=xo_: e.tensor_tensor(out=x1[:], in0=x1[:], in1=xo_[:], op=ALU.add), reads=["x1", xok], writes=["x1"])
                    op("sp", lambda e, tsl=tsl: e.dma_start(out=x1_d[tsl, :], in_=x1[:]), reads=["x1"], writes=["x1_d"], dma=True)
                    if upto == 5 and SUB < 5:
                        continue
                    op("act", lambda e: e.activation(out=hjunk[:], in_=x1[:], func=AF.Square, accum_out=f5[:, 0:1]), reads=["x1"], writes=["hjunk", "f5"])
                    rstd_from_ss(f5[:, 0:1], 1024, f5[:, 1:2], f5[:, 2:3], ["f5"])
                    op("dve", lambda e: e.scalar_tensor_tensor(out=h2[:], in0=x1[:], scalar=f5[:, 1:2], in1=g2[:], op0=ALU.mult, op1=ALU.mult),
                       reads=["x1", "f5", "g2"], writes=["h2"])
                    op("dve", lambda e: e.tensor_tensor(out=h2[:], in0=h2[:], in1=shf[:], op=ALU.add), reads=["h2", "shf"], writes=["h2"])
                    for kc in range(8):
                        bk = 4 + kc // 4
                        op("pe", lambda e, kc=kc, bk=bk: e.transpose(out=ps[:, bk, (kc % 4) * 128:(kc % 4 + 1) * 128], in_=h2[:, kc * 128:(kc + 1) * 128],
                                                                    identity=ident[:]),
                           reads=["h2", "ident"], writes=["ps%d" % bk])
                    srcT = ps[:, 4:6, :].rearrange("p a (b c) -> p (a b) c", c=128)
                    op("act", lambda e, srcT=srcT: e.copy(out=h2Tf[:], in_=srcT), reads=["ps4", "ps5"], writes=["h2Tf"])
                    op("dve", lambda e, tsl=tsl: e.tensor_copy(out=h2T[:, :, tsl], in_=h2Tf[:]), reads=["h2Tf"], writes=["h2T"])
                    if upto == 5 and SUB < 6:
                        continue
                    for kc in range(8):
                        op("pe", lambda e, kc=kc: e.matmul(ps[:, 0, 0:256], lhsT=h2Tf[:, kc, :], rhs=wr[:, kc, :], start=(kc == 0), stop=(kc == 7)),
                           reads=["h2Tf", "wr"], writes=["ps0"])
                    op("act", lambda e: e.activation(out=scr[:], in_=ps[:, 0, 0:256], func=AF.Sigmoid), reads=["ps0"], writes=["scr"])
                    op("dve", lambda e: e.tensor_tensor(out=cho[:], in0=scr[:], in1=rb[:], op=ALU.add), reads=["scr", "rb"], writes=["cho"])
                    for g in range(8):
                        op("dve", lambda e, g=g: e.max(out=m8[:, g, :], in_=cho[:, g * 32:(g + 1) * 32]), reads=["cho"], writes=["m8"])
                    op("dve", lambda e: e.tensor_tensor(out=gs[:], in0=m8[:, :, 0], in1=m8[:, :, 1], op=ALU.add), reads=["m8"], writes=["gs"])
                    op("dve", lambda e: e.max(out=t8[:], in_=gs[:]), reads=["gs"], writes=["t8"])
                    op("dve", lambda e: e.tensor_scalar(out=gmk[:], in0=gs[:], scalar1=t8[:, 3:4], scalar2=None, op0=ALU.is_ge), reads=["gs", "t8"], writes=["gmk"])
                    op("dve", lambda e: e.tensor_scalar(out=pen[:], in0=gmk[:], scalar1=1.0, scalar2=1e30, op0=ALU.subtract, op1=ALU.mult),
                       reads=["gmk"], writes=["pen"])
                    op("dve", lambda e: e.tensor_tensor(out=msk[:].rearrange("p (g k) -> p g k", k=32), in0=cho[:].rearrange("p (g k) -> p g k", k=32),
                                                        in1=gmk[:].unsqueeze(2).to_broadcast([128, 8, 32]), op=ALU.mult), reads=["cho", "gmk"], writes=["msk"])
                    op("dve", lambda e: e.tensor_tensor(out=msk[:].rearrange("p (g k) -> p g k", k=32), in0=msk[:].rearrange("p (g k) -> p g k", k=32),
                                                        in1=pen[:].unsqueeze(2).to_broadcast([128, 8, 32]), op=ALU.add), reads=["msk", "pen"], writes=["msk"])
                    op("dve", lambda e: e.max(out=m8b[:], in_=msk[:]), reads=["msk"], writes=["m8b"])
                    op("dve", lambda e: e.tensor_scalar(out=msk[:], in0=msk[:], scalar1=m8b[:, 7:8], scalar2=None, op0=ALU.is_ge), reads=["msk", "m8b"], writes=["msk"])
                    op("dve", lambda e: e.tensor_tensor(out=msk[:], in0=msk[:], in1=scr[:], op=ALU.mult), reads=["msk", "scr"], writes=["msk"])
                    op("dve", lambda e: e.reduce_sum(out=f5[:, 3:4], in_=msk[:], axis=AX.X), reads=["msk"], writes=["f5"])
                    op("dve", lambda e: e.tensor_scalar(out=f5[:, 3:4], in0=f5[:, 3:4], scalar1=1e-20, scalar2=None, op0=ALU.add), reads=["f5"], writes=["f5"])
                    op("dve", lambda e: e.reciprocal(out=f5[:, 4:5], in_=f5[:, 3:4]), reads=["f5"], writes=["f5"])
                    op("dve", lambda e, t=t: e.tensor_scalar(out=G[:, t, 0:256], in0=msk[:], scalar1=f5[:, 4:5], scalar2=2.5, op0=ALU.mult, op1=ALU.mult),
                       reads=["msk", "f5"], writes=["G"])
            P.barrier()
            if debug:
                dG = nc.dram_tensor("dbg_G", [128, 16, 257], F32, kind="ExternalOutput").ap()
                dH = nc.dram_tensor("dbg_h2T", [128, 8, 2048], BF16, kind="ExternalOutput").ap()
                for t_ in range(16):
                    op("sp", lambda e, t_=t_: e.dma_start(out=dG[:, t_, :], in_=G[:, t_, :]), reads=["G"], dma=True, is_out=True)
                for k_ in range(8):
                    op("sp", lambda e, k_=k_: e.dma_start(out=dH[:, k_, :], in_=h2T[:, k_, :]), reads=["h2T"], dma=True, is_out=True)

            P.enabled = upto >= 6 and (only is None or only == 6)
            accm = SB(stB, "accm", [128, 16, 1024])
            for t_ in range(16):
                op("pool", lambda e, t_=t_: e.memset(accm[:, t_, :], 0.0), writes=[("acc", t_)])
            with contextlib.ExitStack() as st:
                sg_f = [SB(st, "sgf0", [128, 8, 256])] * 2
                su_f = [SB(st, "suf0", [128, 8, 256])] * 2
                sd_f = [SB(st, "sdf0", [128, 2, 1024])] * 2
                wgb = [SB(st, "wgb%d" % i, [128, 8, 256], BF16) for i in range(2)]
                wub = [SB(st, "wub%d" % i, [128, 8, 256], BF16) for i in range(2)]
                wdb = [SB(st, "wdb%d" % i, [128, 2, 1024], BF16) for i in range(2)]
                sgt = [SB(st, "sgt%d" % i, [128, 1024]) for i in range(2)]
                actb = [SB(st, "actb%d" % i, [128, 2, 512], BF16) for i in range(2)]
                di = [0]

                def emit_load(ex):
                    b = ex % 2
                    op("sp", lambda e, ex=ex, b=b: e.dma_start(out=sg_f[b][:], in_=weg[ex].rearrange("(kc p) n -> p kc n", p=128)), writes=["sgf0"], dma=True)
                    op("sp", lambda e, ex=ex, b=b: e.dma_start(out=su_f[b][:], in_=weu[ex].rearrange("(kc p) n -> p kc n", p=128)), writes=["suf0"], dma=True)
                    op("sp", lambda e, ex=ex, b=b: e.dma_start(out=sd_f[b][:], in_=wed[ex].rearrange("(kc p) n -> p kc n", p=128)), writes=["sdf0"], dma=True)
                    op("pool", lambda e, b=b: e.tensor_copy(out=wgb[b][:], in_=sg_f[b][:]), reads=["sgf0"], writes=["wgb%d" % b])
                    op("pool", lambda e, b=b: e.tensor_copy(out=wub[b][:], in_=su_f[b][:]), reads=["suf0"], writes=["wub%d" % b])
                    op("pool", lambda e, b=b: e.tensor_copy(out=wdb[b][:], in_=sd_f[b][:]), reads=["sdf0"], writes=["wdb%d" % b])

                def emit_gu(ex, tq, ab):
                    b = ex % 2
                    qsl = slice(tq * 512, (tq + 1) * 512)
                    for fb in range(2):
                        for kc in range(8):
                            op("pe", lambda e, kc=kc, fb=fb, b=b, qsl=qsl: e.matmul(ps[:, fb, :], lhsT=wgb[b][:, kc, fb * 128:(fb + 1) * 128], rhs=h2T[:, kc, qsl],
                                                                                   start=(kc == 0), stop=(kc == 7)),
                               reads=["wgb%d" % b, "h2T"], writes=["ps%d" % fb])
                    for fb in range(2):
                        for kc in range(8):
                            op("pe", lambda e, kc=kc, fb=fb, b=b, qsl=qsl: e.matmul(ps[:, 2 + fb, :], lhsT=wub[b][:, kc, fb * 128:(fb + 1) * 128], rhs=h2T[:, kc, qsl],
                                                                                   start=(kc == 0), stop=(kc == 7)),
                               reads=["wub%d" % b, "h2T"], writes=["ps%d" % (2 + fb)])
                    op("act", lambda e, ab=ab: e.activation(out=sgt[ab][:].rearrange("p (a b) -> p a b", b=512), in_=ps[:, 0:2, :], func=AF.Silu),
                       reads=["ps0", "ps1"], writes=["sgt%d" % ab])
                    op("dve", lambda e, ab=ab: e.tensor_tensor(out=actb[ab][:], in0=sgt[ab][:].rearrange("p (a b) -> p a b", b=512), in1=ps[:, 2:4, :], op=ALU.mult),
                       reads=["sgt%d" % ab, "ps2", "ps3"], writes=["actb%d" % ab])

                def emit_down(ex, tq, ab):
                    b = ex % 2
                    gcol = ex if ex < n_exp else 256
                    for tt in range(4):
                        t = tq * 4 + tt
                        pd = 4 + 2 * (di[0] % 2)
                        di[0] += 1
                        for half in range(2):
                            for fb in range(2):
                                op("pe", lambda e, fb=fb, half=half, tt=tt, ab=ab, b=b, pd=pd: e.matmul(
                                    ps[:, pd + half, :], lhsT=actb[ab][:, fb, tt * 128:(tt + 1) * 128], rhs=wdb[b][:, fb, half * 512:(half + 1) * 512],
                                    start=(fb == 0), stop=(fb == 1)),
                                   reads=["actb%d" % ab, "wdb%d" % b], writes=["ps%d" % (pd + half)])
                        op("dve", lambda e, t=t, gcol=gcol, pd=pd: e.scalar_tensor_tensor(
                            out=accm[:, t, :].rearrange("p (a b) -> p a b", b=512), in0=ps[:, pd:pd + 2, :], scalar=G[:, t, gcol:gcol + 1],
                            in1=accm[:, t, :].rearrange("p (a b) -> p a b", b=512), op0=ALU.mult, op1=ALU.add),
                           reads=["ps%d" % pd, "ps%d" % (pd + 1), "G", ("acc", t)], writes=[("acc", t)])

                units = [(ex, tq) for ex in range(n_exp + 1) for tq in range(4)]
                prev = None
                for ui, (ex, tq) in enumerate(units):
                    if tq == 0:
                        emit_load(ex)
                    ab = ui % 2
                    emit_gu(ex, tq, ab)
                    if prev is not None:
                        emit_down(*prev)
                    prev = (ex, tq, ab)
                emit_down(*prev)
                P.barrier()
            P.enabled = upto >= 7 and (only is None or only == 7)
            with contextlib.ExitStack() as st:
                gf = SB(st, "gf", [128, 1024])
                mod_bc(gf[:], 5, "gf")
                nfin = SB(st, "nfin", [128, 1024])
                bcast_load(nfin[:], norm_final, "nfin")
                x1b = [SB(st, "x1b%d" % i, [128, 1024]) for i in range(2)]
                ob = [SB(st, "ob%d" % i, [128, 1024]) for i in range(2)]
                fj = SB(st, "fj", [128, 1024], BF16)
                f6 = SB(st, "f6", [128, 4])
                for t in range(16):
                    tsl = slice(t * 128, (t + 1) * 128)
                    xb_ = x1b[t % 2]
                    xbk = "x1b%d" % (t % 2)
                    o_ = ob[t % 2]
                    obk = "ob%d" % (t % 2)
                    op("sp", lambda e, xb_=xb_, tsl=tsl: e.dma_start(out=xb_[:], in_=x1_d[tsl, :]), reads=["x1_d"], writes=[xbk], dma=True)
                    op("dve", lambda e, t=t, o_=o_: e.tensor_tensor(out=o_[:], in0=accm[:, t, :], in1=gf[:], op=ALU.mult), reads=[("acc", t), "gf"], writes=[obk])
                    op("dve", lambda e, o_=o_, xb_=xb_: e.tensor_tensor(out=o_[:], in0=o_[:], in1=xb_[:], op=ALU.add), reads=[obk, xbk], writes=[obk])
                    op("act", lambda e, o_=o_: e.activation(out=fj[:], in_=o_[:], func=AF.Square, accum_out=f6[:, 0:1]), reads=[obk], writes=["fj", "f6"])
                    rstd_from_ss(f6[:, 0:1], 1024, f6[:, 1:2], f6[:, 2:3], ["f6"])
                    op("dve", lambda e, o_=o_: e.scalar_tensor_tensor(out=o_[:], in0=o_[:], scalar=f6[:, 1:2], in1=nfin[:], op0=ALU.mult, op1=ALU.mult),
                       reads=[obk, "f6", "nfin"], writes=[obk])
                    op("sp", lambda e, o_=o_, tsl=tsl: e.dma_start(out=y_out[tsl, :], in_=o_[:]), reads=[obk], dma=True, is_out=True)
        P.enabled = True
        if debug:
            P.barrier()
            for nm_, src_ in [("hT_d", hT_d), ("ropeC", ropeC), ("ropeS", ropeS), ("yaT_d", yaT_d), ("ypre_d", ypre_d), ("ps_d", ps_d), ("x1_d", x1_d), ("modrow", modrow)]:
                dst_ = nc.dram_tensor("dbg_" + nm_, list(src_.shape), src_.dtype, kind="ExternalOutput").ap()
                op("sp", lambda e, dst_=dst_, src_=src_: e.dma_start(out=dst_, in_=src_), dma=True, is_out=True)
        P.emit()
    return nc


def _prep_inputs(inp, n_exp=NEXP):
    f = lambda a: np.ascontiguousarray(np.asarray(a, dtype=np.float32))
    x = f(inp["x"])
    c = f(inp["c"])
    pos = np.asarray(inp["positions"]).astype(np.int32)
    w_in = f(inp["w_in"])[0]
    sizes = [1024, 1024, 1024, 2048, 3072, 32, 2048]
    offs = np.cumsum([0] + sizes)
    wq, wk, wv, wz, wxbc, wdt, wg = [w_in[:, offs[i]:offs[i + 1]] for i in range(7)]
    perm64 = np.arange(64)
    perm64[:8] = np.arange(8, 16)
    perm64[8:16] = np.arange(0, 8)
    perm128 = np.concatenate([perm64, 64 + perm64])
    wqk = np.empty((8, 5, 1024, 128), np.float32)
    for h in range(8):
        qh = wq[:, h * 128:(h + 1) * 128]
        kh = wk[:, h * 128:(h + 1) * 128]
        wqk[h, 0] = qh
        wqk[h, 1] = qh[:, perm128]
        wqk[h, 2] = kh
        wqk[h, 3] = kh[:, perm128]
        wqk[h, 4] = wv[:, h * 128:(h + 1) * 128]
    conv_w = f(inp["conv_w"])[0]
    conv_b = f(inp["conv_b"])[0]
    convw = np.ascontiguousarray(conv_w.T.reshape(24, 128, 4).transpose(1, 0, 2))
    convb = np.ascontiguousarray(conv_b.reshape(24, 128).T)
    weg = np.concatenate([f(inp["w_exp_gate"])[0][:n_exp], f(inp["w_sh_gate"])], axis=0)
    weu = np.concatenate([f(inp["w_exp_up"])[0][:n_exp], f(inp["w_sh_up"])], axis=0)
    wed = np.concatenate([f(inp["w_exp_down"])[0][:n_exp], f(inp["w_sh_down"])], axis=0)
    ident = np.eye(128, dtype=np.float32)
    tri = np.triu(np.ones((128, 128), np.float32))
    ones = np.ones((128, 128), np.float32)
    cmask = np.zeros((4, 128, 512), np.float32)
    kk = np.arange(128)[:, None]
    qq = np.arange(512)[None, :]
    for d in range(4):
        cmask[d] = (d * 128 + kk <= qq)
    invf = np.zeros((128, 2), np.float32)
    inv_freq = (1.0 / (np.float32(500000.0) ** (np.arange(0, 16, 2, dtype=np.float32) / np.float32(16)))).astype(np.float32)
    for p in range(128):
        dd = p % 64
        if dd < 16:
            invf[p, 0] = inv_freq[dd % 8]
            invf[p, 1] = -1.0 if dd < 8 else 1.0
    shared = {
        "w_ada": f(inp["w_ada"])[0], "b_ada": f(inp["b_ada"]), "norm_mix": f(inp["norm_mix"]), "norm_ffn": f(inp["norm_ffn"]),
        "norm_final": f(inp["norm_final"]).reshape(1, 1024), "wqk": wqk, "wz": np.ascontiguousarray(wz), "wxbc": np.ascontiguousarray(wxbc),
        "wdt": np.ascontiguousarray(wdt), "wg": np.ascontiguousarray(wg), "b_gate": f(inp["b_gate"]),
        "lamv": np.stack([f(inp["lambda_q1"])[0], f(inp["lambda_k1"])[0], f(inp["lambda_q2"])[0], f(inp["lambda_k2"])[0]]),
        "ahn": f(inp["attn_head_norm"]), "convw": convw, "convb": convb, "dt_bias": f(inp["dt_bias"]), "a_log": f(inp["a_log"]),
        "d_skip": f(inp["d_skip"]), "ssd_norm": f(inp["ssd_norm"]), "wba": f(inp["w_branch_attn"])[0], "wbs": f(inp["w_branch_ssd"])[0],
        "wo": f(inp["w_out"])[0], "wr": f(inp["w_router"])[0], "rbias": f(inp["router_bias"]), "weg": weg, "weu": weu, "wed": wed,
        "ident": ident, "tri": tri, "ones": ones, "cmask": cmask, "invf": invf,
    }
    in_maps = []
    for core in range(8):
        b, j = core // 4, core % 4
        npad = 6144 - 2048 * j
        xl = np.zeros((8192, 1024), np.float32)
        xl[npad:] = x[b, :2048 * (j + 1)]
        valid = np.zeros(8192, np.float32)
        valid[npad:] = 1.0
        posl = np.zeros(8192, np.int32)
        posl[npad:] = pos[b, :2048 * (j + 1)]
        m = dict(shared)
        m["xl"] = xl
        m["validb"] = np.ascontiguousarray(np.broadcast_to(valid[None, :], (128, 8192)))
        m["valid_tm"] = np.ascontiguousarray(valid.reshape(64, 128).T)
        m["posb"] = np.ascontiguousarray(np.broadcast_to(posl[None, :], (128, 8192)))
        m["c_col"] = np.ascontiguousarray(c[b].reshape(8, 128).T)
        in_maps.append(m)
    return in_maps


_NC_CACHE = {}


def kernel(**inputs):
    in_maps = _prep_inputs(inputs)
    if "nc" not in _NC_CACHE:
        _NC_CACHE["nc"] = build_nc()
    nc = _NC_CACHE["nc"]
    res = run_bass_kernel_spmd(nc, in_maps, core_ids=list(range(8)))
    out = np.empty((2, 8192, 1024), np.float32)
    for core in range(8):
        b, j = core // 4, core % 4
        out[b, 2048 * j:2048 * (j + 1)] = res.results[core]["y_out"]
    return out
```

```python
import contextlib
import math
import numpy as np
import ml_dtypes
import concourse.bass as bass
import concourse.mybir as mybir
from concourse.bass_utils import run_bass_kernel_spmd

F32 = mybir.dt.float32
BF16 = mybir.dt.bfloat16
I32 = mybir.dt.int32
AF = mybir.ActivationFunctionType
ALU = mybir.AluOpType
AX = mybir.AxisListType

ENGS = ["pe", "act", "dve", "pool", "sp"]
NDMA = 24
EPS = 1e-6
TWO_PI = 6.283185307179586
NEXP = 256
DEBUG = False
import os
SUB = int(os.environ.get('KSUB', '9'))


class Prog:
    def __init__(self, nc):
        self.nc = nc
        self.ops = {e: [] for e in ENGS}
        self.count = {e: 0 for e in ENGS}
        self.last_w = {}
        self.readers = {}
        self.waited = {e: {} for e in ENGS}
        self.pending = {e: [] for e in ENGS}
        self.dma_i = 0
        self.dma_last = {}
        self.out_deps = []
        self.enabled = True

    def _need(self, eng, dep, waits):
        if dep is None:
            return
        k, v = dep
        if k == "pe" and eng == "pe":
            return
        if self.waited[eng].get(k, 0) >= v:
            return
        self.waited[eng][k] = v
        waits.append((k, v))

    def barrier(self):
        deps = [(e, self.count[e]) for e in ENGS if self.count[e] > 0]
        deps += [(k, v) for k, v in self.dma_last.items()]
        for e in ENGS:
            self.pending[e] = list(deps)

    def op(self, eng, fn, reads=(), writes=(), dma=False, is_out=False):
        if not self.enabled:
            return None
        waits = []
        for d in self.pending[eng]:
            self._need(eng, d, waits)
        self.pending[eng] = []
        for k in reads:
            self._need(eng, self.last_w.get(k), waits)
        for k in writes:
            self._need(eng, self.last_w.get(k), waits)
            for d in self.readers.get(k, ()):
                self._need(eng, d, waits)
        if dma:
            s = self.dma_i % NDMA
            n = self.dma_i // NDMA
            if n > 0:
                self._need(eng, (("dma", s), 16 * n), waits)
            dep = (("dma", s), 16 * (n + 1))
            inc = (("dma", s), 16)
            self.dma_last[("dma", s)] = 16 * (n + 1)
            self.dma_i += 1
        else:
            self.count[eng] += 1
            dep = (eng, self.count[eng])
            inc = (eng, 1)
        m = {}
        for k, v in waits:
            m[k] = max(m.get(k, 0), v)
        self.ops[eng].append((list(m.items()), fn, inc))
        for k in reads:
            self.readers.setdefault(k, []).append(dep)
        for k in writes:
            self.last_w[k] = dep
            self.readers[k] = []
        if is_out:
            self.out_deps.append(dep)
        return dep

    def emit(self):
        nc = self.nc
        waits = []
        for d in self.out_deps:
            self._need("sp", d, waits)
        m = {}
        for k, v in waits:
            m[k] = max(m.get(k, 0), v)
        final_waits = list(m.items())
        with contextlib.ExitStack() as st:
            sems = {}
            for e in ENGS:
                sems[e] = st.enter_context(nc.semaphore("s_" + e))
            for i in range(NDMA):
                sems[("dma", i)] = st.enter_context(nc.semaphore("s_dma%d" % i))
            block = st.enter_context(nc.Block())

            def run(engname):
                def body(eng):
                    for waits_, fn, inc in self.ops[engname]:
                        for k, v in waits_:
                            eng.wait_ge(sems[k], v)
                        ins = fn(eng)
                        ins.then_inc(sems[inc[0]], inc[1])
                    if engname == "sp":
                        for k, v in final_waits:
                            eng.wait_ge(sems[k], v)
                return body

            block.tensor(run("pe"))
            block.scalar(run("act"))
            block.vector(run("dve"))
            block.gpsimd(run("pool"))
            block.sync(run("sp"))


def build_nc(n_exp=NEXP, debug=False, upto=99, only=None):
    nc = bass.Bass("TRN2", target_bir_lowering=False)
    P = Prog(nc)
    op = P.op

    def din(name, shape, dt=F32):
        return nc.dram_tensor(name, list(shape), dt, kind="ExternalInput").ap()

    def dscr(name, shape, dt=F32):
        return nc.dram_tensor(name, list(shape), dt).ap()

    xl = din("xl", [8192, 1024])
    validb = din("validb", [128, 8192])
    valid_tm_d = din("valid_tm", [128, 64])
    posb = din("posb", [128, 8192], I32)
    c_col = din("c_col", [128, 8])
    w_ada = din("w_ada", [1024, 6144])
    b_ada = din("b_ada", [1, 6144])
    norm_mix = din("norm_mix", [1, 1024])
    norm_ffn = din("norm_ffn", [1, 1024])
    norm_final = din("norm_final", [1, 1024])
    wqk = din("wqk", [8, 5, 1024, 128])
    wz_d = din("wz", [1024, 2048])
    wxbc_d = din("wxbc", [1024, 3072])
    wdt_d = din("wdt", [1024, 32])
    wg_d = din("wg", [1024, 2048])
    b_gate = din("b_gate", [1, 2048])
    lam_d = din("lamv", [4, 64])
    ahn = din("ahn", [1, 128])
    convw_d = din("convw", [128, 24, 4])
    convb_d = din("convb", [128, 24])
    dtb_d = din("dt_bias", [1, 32])
    alog_d = din("a_log", [1, 32])
    dskip_d = din("d_skip", [1, 32])
    ssdn_d = din("ssd_norm", [1, 2048])
    wba_d = din("wba", [1024, 1024])
    wbs_d = din("wbs", [2048, 1024])
    wo_d = din("wo", [1024, 1024])
    wr_d = din("wr", [1024, 256])
    rbias_d = din("rbias", [1, 256])
    weg = din("weg", [n_exp + 1, 1024, 256])
    weu = din("weu", [n_exp + 1, 1024, 256])
    wed = din("wed", [n_exp + 1, 256, 1024])
    ident_d = din("ident", [128, 128])
    tri_d = din("tri", [128, 128])
    ones_d = din("ones", [128, 128])
    cmask_d = din("cmask", [4, 128, 512])
    invf_d = din("invf", [128, 2])
    y_out = nc.dram_tensor("y_out", [2048, 1024], F32, kind="ExternalOutput").ap()

    modrow = dscr("modrow", [1, 6144])
    hT_d = dscr("hT_d", [128, 8, 8192], BF16)
    ropeC = dscr("ropeC", [128, 8192])
    ropeS = dscr("ropeS", [128, 8192])
    yaT_d = dscr("yaT_d", [128, 8, 2048], BF16)
    ypre_d = dscr("ypre_d", [2048, 2048])
    ps_d = dscr("ps_d", [2048, 1024])
    x1_d = dscr("x1_d", [2048, 1024])

    dbg = {}

    with contextlib.ExitStack() as top:
        _cnt = [0]

        def SB(st, name, shape, dt=F32):
            _cnt[0] += 1
            return st.enter_context(nc.sbuf_tensor("sb%d_%s" % (_cnt[0], name), list(shape), dt))

        ps = top.enter_context(nc.psum_tensor("ps", [128, 8, 512], F32))
        ident = SB(top, "ident", [128, 128])
        tri = SB(top, "tri", [128, 128])
        ones = SB(top, "ones", [128, 128])
        vtm = SB(top, "vtm", [128, 64])
        small = SB(top, "small", [128, 64])
        op("sp", lambda e: e.dma_start(out=ident[:], in_=ident_d), writes=["ident"], dma=True)
        op("sp", lambda e: e.dma_start(out=tri[:], in_=tri_d), writes=["tri"], dma=True)
        op("sp", lambda e: e.dma_start(out=ones[:], in_=ones_d), writes=["ones"], dma=True)
        op("sp", lambda e: e.dma_start(out=vtm[:], in_=valid_tm_d), writes=["vtm"], dma=True)

        def bank(i):
            return ps[:, i, :]

        def bcast_load(dst, src_row, key):
            op("sp", lambda e: e.dma_start(out=dst, in_=src_row.partition_broadcast(128)), writes=[key], dma=True)

        def rstd_from_ss(ss_ap, n, out_ap, tmp_ap, keys):
            op("dve", lambda e: e.tensor_scalar(out=tmp_ap, in0=ss_ap, scalar1=1.0 / n, scalar2=EPS, op0=ALU.mult, op1=ALU.add),
               reads=keys, writes=keys)
            op("act", lambda e: e.sqrt(out=tmp_ap, in_=tmp_ap), reads=keys, writes=keys)
            op("dve", lambda e: e.reciprocal(out=out_ap, in_=tmp_ap), reads=keys, writes=keys)

        P.enabled = only is None
        with contextlib.ExitStack() as st:
            cc_t = SB(st, "cc_t", [128, 8])
            sc_t = SB(st, "sc_t", [128, 8])
            modr = SB(st, "modr", [1, 6144])
            bada = SB(st, "bada", [1, 6144])
            wst = [SB(st, "wst%d" % i, [128, 8, 512]) for i in range(2)]
            op("sp", lambda e: e.dma_start(out=cc_t[:], in_=c_col), writes=["cc"], dma=True)
            op("sp", lambda e: e.dma_start(out=bada[:], in_=b_ada), writes=["bada"], dma=True)
            op("act", lambda e: e.activation(out=sc_t[:], in_=cc_t[:], func=AF.Silu), reads=["cc"], writes=["sc"])
            for cb in range(12):
                w = wst[cb % 2]
                wk = "wst%d" % (cb % 2)
                op("sp", lambda e, w=w, cb=cb: e.dma_start(
                    out=w[:], in_=w_ada[:, cb * 512:(cb + 1) * 512].rearrange("(kc p) n -> p kc n", p=128)),
                   writes=[wk], dma=True)
                for kc in range(8):
                    op("pe", lambda e, w=w, kc=kc: e.matmul(ps[0:1, 0, :], lhsT=sc_t[:, kc:kc + 1], rhs=w[:, kc, :],
                                                         start=(kc == 0), stop=(kc == 7)),
                       reads=[wk, "sc"], writes=["ps0"])
                op("dve", lambda e, cb=cb: e.tensor_tensor(out=modr[0:1, cb * 512:(cb + 1) * 512], in0=ps[0:1, 0, :],
                                                        in1=bada[0:1, cb * 512:(cb + 1) * 512], op=ALU.add),
                   reads=["ps0", "bada"], writes=["modr"])
            op("sp", lambda e: e.dma_start(out=modrow, in_=modr[:]), reads=["modr"], writes=["modrow"], dma=True)
        P.barrier()

        def mod_bc(dst, idx, key):
            op("sp", lambda e: e.dma_start(out=dst, in_=modrow[0:1, idx * 1024:(idx + 1) * 1024].partition_broadcast(128)),
               reads=["modrow"], writes=[key], dma=True)

        P.enabled = upto >= 1 and (only is None or only == 1)
        with contextlib.ExitStack() as st:
            g1 = SB(st, "g1", [128, 1024])
            shm = SB(st, "shm", [128, 1024])
            nm = SB(st, "nm", [128, 1024])
            bcast_load(nm[:], norm_mix, "nm")
            mod_bc(shm[:], 0, "shm")
            mod_bc(g1[:], 1, "g1")
            op("dve", lambda e: e.scalar_tensor_tensor(out=g1[:], in0=g1[:], scalar=1.0, in1=nm[:], op0=ALU.add, op1=ALU.mult),
               reads=["g1", "nm"], writes=["g1"])
            xt = [SB(st, "xt%d" % i, [128, 1024]) for i in range(2)]
            hb = [SB(st, "hb%d" % i, [128, 1024]) for i in range(2)]
            junk = SB(st, "junk", [128, 1024], BF16)
            hst = [SB(st, "hst%d" % i, [128, 8, 512], BF16) for i in range(2)]
            ssb = SB(st, "ssb", [128, 4])
            for stile in range(16):
                hs = hst[stile % 2]
                hk = "hst%d" % (stile % 2)
                for tt in range(4):
                    t = stile * 4 + tt
                    x_ = xt[t % 2]
                    xk = "xt%d" % (t % 2)
                    h_ = hb[t % 2]
                    hbk = "hb%d" % (t % 2)
                    op("sp", lambda e, x_=x_, t=t: e.dma_start(out=x_[:], in_=xl[t * 128:(t + 1) * 128, :]), writes=[xk], dma=True)
                    op("act", lambda e, x_=x_: e.activation(out=junk[:], in_=x_[:], func=AF.Square, accum_out=ssb[:, 0:1]),
                       reads=[xk], writes=["junk", "ssb"])
                    rstd_from_ss(ssb[:, 0:1], 1024, ssb[:, 1:2], ssb[:, 2:3], ["ssb"])
                    op("dve", lambda e, t=t: e.tensor_tensor(out=ssb[:, 1:2], in0=ssb[:, 1:2], in1=vtm[:, t:t + 1], op=ALU.mult),
                       reads=["ssb", "vtm"], writes=["ssb"])
                    op("dve", lambda e, x_=x_, h_=h_: e.scalar_tensor_tensor(out=h_[:], in0=x_[:], scalar=ssb[:, 1:2], in1=g1[:],
                                                                          op0=ALU.mult, op1=ALU.mult),
                       reads=[xk, "ssb", "g1"], writes=[hbk])
                    op("dve", lambda e, h_=h_, t=t: e.scalar_tensor_tensor(out=h_[:], in0=shm[:], scalar=vtm[:, t:t + 1], in1=h_[:],
                                                                           op0=ALU.mult, op1=ALU.add),
                       reads=[hbk, "shm", "vtm"], writes=[hbk])
                    b0 = (t % 2) * 2
                    for kc in range(8):
                        bk = b0 + kc // 4
                        op("pe", lambda e, h_=h_, kc=kc, bk=bk: e.transpose(out=ps[:, bk, (kc % 4) * 128:(kc % 4 + 1) * 128],
                                                                          in_=h_[:, kc * 128:(kc + 1) * 128], identity=ident[:]),
                           reads=[hbk, "ident"], writes=["ps%d" % bk])
                    op("act", lambda e, hs=hs, tt=tt, b0=b0: e.copy(
                        out=hs[:, :, tt * 128:(tt + 1) * 128],
                        in_=ps[:, b0:b0 + 2, :].rearrange("p a (b c) -> p (a b) c", c=128)),
                       reads=["ps%d" % b0, "ps%d" % (b0 + 1)], writes=[hk])
                op("sp", lambda e, hs=hs, stile=stile: e.dma_start(out=hT_d[:, :, stile * 512:(stile + 1) * 512], in_=hs[:]),
                   reads=[hk], writes=["hT_d"], dma=True)
            invf = SB(st, "invf", [128, 2])
            op("sp", lambda e: e.dma_start(out=invf[:], in_=invf_d), writes=["invf"], dma=True)
            pi_t = SB(st, "pi_t", [128, 512], I32)
            th = SB(st, "th", [128, 512])
            tf = SB(st, "tf", [128, 512])
            ti = SB(st, "ti", [128, 512], I32)
            rr = SB(st, "rr", [128, 512])
            for stile in range(16):
                sl = slice(stile * 512, (stile + 1) * 512)
                op("sp", lambda e, sl=sl: e.dma_start(out=pi_t[:], in_=posb[:, sl]), writes=["pi"], dma=True)
                for which in range(2):
                    op("dve", lambda e: e.tensor_copy(out=th[:], in_=pi_t[:]), reads=["pi"], writes=["th"])
                    op("dve", lambda e, which=which: e.tensor_scalar(out=th[:], in0=th[:], scalar1=invf[:, 0:1],
                                                                     scalar2=(0.0 if which == 0 else math.pi / 2),
                                                                     op0=ALU.mult, op1=ALU.add),
                       reads=["th", "invf"], writes=["th"])
                    op("dve", lambda e: e.tensor_scalar(out=tf[:], in0=th[:], scalar1=1.0 / TWO_PI, scalar2=None, op0=ALU.mult),
                       reads=["th"], writes=["tf"])
                    op("dve", lambda e: e.tensor_copy(out=ti[:], in_=tf[:]), reads=["tf"], writes=["ti"])
                    op("dve", lambda e: e.tensor_copy(out=tf[:], in_=ti[:]), reads=["ti"], writes=["tf"])
                    op("dve", lambda e: e.scalar_tensor_tensor(out=th[:], in0=tf[:], scalar=-TWO_PI, in1=th[:], op0=ALU.mult, op1=ALU.add),
                       reads=["tf", "th"], writes=["th"])
                    op("dve", lambda e: e.tensor_scalar(out=th[:], in0=th[:], scalar1=-math.pi, scalar2=math.pi, op0=ALU.max, op1=ALU.min),
                       reads=["th"], writes=["th"])
                    op("act", lambda e: e.activation(out=rr[:], in_=th[:], func=AF.Sin), reads=["th"], writes=["rr"])
                    if which == 0:
                        op("dve", lambda e: e.tensor_scalar(out=rr[:], in0=rr[:], scalar1=invf[:, 1:2], scalar2=None, op0=ALU.mult),
                           reads=["rr", "invf"], writes=["rr"])
                        op("sp", lambda e, sl=sl: e.dma_start(out=ropeS[:, sl], in_=rr[:]), reads=["rr"], writes=["ropeS"], dma=True)
                    else:
                        op("sp", lambda e, sl=sl: e.dma_start(out=ropeC[:, sl], in_=rr[:]), reads=["rr"], writes=["ropeC"], dma=True)
        P.barrier()

        P.enabled = upto >= 2 and (only is None or only == 2)
        with contextlib.ExitStack() as st:
            cmf = SB(st, "cmf", [128, 512])
            cmb = SB(st, "cmb", [128, 4, 512], BF16)
            for d in range(4):
                op("sp", lambda e, d=d: e.dma_start(out=cmf[:], in_=cmask_d[d]), writes=["cmf"], dma=True)
                op("dve", lambda e, d=d: e.tensor_copy(out=cmb[:, d, :], in_=cmf[:]), reads=["cmf"], writes=["cmb"])
            lv = SB(st, "lv", [128, 4, 64])
            for i in range(4):
                bcast_load(lv[:, i, :], lam_d[i:i + 1, :], "lv")
            lp = SB(st, "lp", [128, 2, 64])
            op("dve", lambda e: e.tensor_tensor(out=lp[:, 0, :], in0=lv[:, 0, :], in1=lv[:, 1, :], op=ALU.mult), reads=["lv"], writes=["lp"])
            op("dve", lambda e: e.tensor_tensor(out=lp[:, 1, :], in0=lv[:, 2, :], in1=lv[:, 3, :], op=ALU.mult), reads=["lv"], writes=["lp"])
            op("dve", lambda e: e.reduce_sum(out=small[:, 0:2], in_=lp[:], axis=AX.X), reads=["lp"], writes=["small"])
            op("act", lambda e: e.activation(out=small[:, 2:4], in_=small[:, 0:2], func=AF.Exp), reads=["small"], writes=["small"])
            op("dve", lambda e: e.scalar_tensor_tensor(out=small[:, 4:5], in0=small[:, 3:4], scalar=-0.2, in1=small[:, 2:3],
                                                       op0=ALU.add, op1=ALU.subtract), reads=["small"], writes=["small"])
            nlam = small[:, 4:5]
            hg = SB(st, "hg", [128, 128])
            bcast_load(hg[:], ahn, "hg")
            op("dve", lambda e: e.tensor_scalar(out=hg[:], in0=hg[:], scalar1=0.8, scalar2=None, op0=ALU.mult), reads=["hg"], writes=["hg"])

            wsf = [SB(st, "wsf%d" % i, [128, 8, 128]) for i in range(2)]
            W5 = [SB(st, "W5_%d" % i, [128, 5, 8, 128], BF16) for i in range(2)]
            hsb = [SB(st, "hsb%d" % i, [128, 8, 512], BF16) for i in range(2)]
            Ct = [SB(st, "Ct%d" % i, [128, 512]) for i in range(2)]
            St = [SB(st, "St%d" % i, [128, 512]) for i in range(2)]
            t1 = SB(st, "t1", [128, 512])
            t2 = SB(st, "t2", [128, 512])
            kT = SB(st, "kT", [128, 8192], BF16)
            qT = SB(st, "qT", [128, 2048], BF16)
            va = SB(st, "va", [128, 64, 130], BF16)
            E = [[SB(st, "E%d_%d" % (c, i), [128, 512], BF16) for i in range(2)] for c in range(2)]
            o0 = SB(st, "o0", [128, 128])
            o1 = SB(st, "o1", [128, 128])
            ojunk = SB(st, "ojunk", [128, 128])
            yst = SB(st, "yst", [128, 512], BF16)
            fs = SB(st, "fs", [128, 8])
            acc = [ps[:, 4 + 2 * c:6 + 2 * c, :].rearrange("p a (b c) -> p (a b) c", c=256) for c in range(2)]
            acck = [["ps4", "ps5"], ["ps6", "ps7"]]
            op("dve", lambda e: e.tensor_copy(out=va[:, :, 128], in_=vtm[:]), reads=["vtm"], writes=["va"])

            for h in range(8):
                Wb = W5[h % 2]
                Wk = "W5_%d" % (h % 2)
                for i in range(5):
                    wi = h * 5 + i
                    ws_ = wsf[wi % 2]
                    wsk = "wsf%d" % (wi % 2)
                    op("sp", lambda e, ws_=ws_, i=i, h=h: e.dma_start(out=ws_[:], in_=wqk[h, i].rearrange("(kc p) n -> p kc n", p=128)),
                       writes=[wsk], dma=True)
                    op("pool", lambda e, ws_=ws_, i=i, Wb=Wb: e.tensor_copy(out=Wb[:, i, :, :], in_=ws_[:]), reads=[wsk], writes=[Wk])
                for stile in range(16):
                    sl = slice(stile * 512, (stile + 1) * 512)
                    hs = hsb[stile % 2]
                    hk = "hsb%d" % (stile % 2)
                    C_ = Ct[stile % 2]
                    S_ = St[stile % 2]
                    ck = "Ct%d" % (stile % 2)
                    sk = "St%d" % (stile % 2)
                    op("sp", lambda e, hs=hs, sl=sl: e.dma_start(out=hs[:], in_=hT_d[:, :, sl]), reads=["hT_d"], writes=[hk], dma=True)
                    op("sp", lambda e, C_=C_, sl=sl: e.dma_start(out=C_[:], in_=ropeC[:, sl]), reads=["ropeC"], writes=[ck], dma=True)
                    op("sp", lambda e, S_=S_, sl=sl: e.dma_start(out=S_[:], in_=ropeS[:, sl]), reads=["ropeS"], writes=[sk], dma=True)
                    jobs = [(2, 3, kT, sl, "kT")]
                    if stile >= 12:
                        jobs.append((0, 1, qT, slice((stile - 12) * 512, (stile - 11) * 512), "qT"))
                    for (ia, ib, dst, dsl, dk) in jobs:
                        for kc in range(8):
                            op("pe", lambda e, kc=kc, ia=ia, hs=hs, Wb=Wb: e.matmul(ps[:, 0, :], lhsT=Wb[:, ia, kc, :], rhs=hs[:, kc, :],
                                                                                   start=(kc == 0), stop=(kc == 7)),
                               reads=[Wk, hk], writes=["ps0"])
                        for kc in range(8):
                            op("pe", lambda e, kc=kc, ib=ib, hs=hs, Wb=Wb: e.matmul(ps[:, 1, :], lhsT=Wb[:, ib, kc, :], rhs=hs[:, kc, :],
                                                                                   start=(kc == 0), stop=(kc == 7)),
                               reads=[Wk, hk], writes=["ps1"])
                        op("dve", lambda e, C_=C_: e.tensor_tensor(out=t1[:], in0=ps[:, 0, :], in1=C_[:], op=ALU.mult),
                           reads=["ps0", ck], writes=["t1"])
                        op("dve", lambda e, S_=S_: e.tensor_tensor(out=t2[:], in0=ps[:, 1, :], in1=S_[:], op=ALU.mult),
                           reads=["ps1", sk], writes=["t2"])
                        op("dve", lambda e, dst=dst, dsl=dsl: e.tensor_tensor(out=dst[:, dsl], in0=t1[:], in1=t2[:], op=ALU.add),
                           reads=["t1", "t2"], writes=[dk])
                    for tt in range(4):
                        for kc in range(8):
                            op("pe", lambda e, kc=kc, tt=tt, hs=hs, Wb=Wb: e.matmul(ps[:, 2, tt * 128:(tt + 1) * 128],
                                                                                   lhsT=hs[:, kc, tt * 128:(tt + 1) * 128], rhs=Wb[:, 4, kc, :],
                                                                                   start=(kc == 0), stop=(kc == 7)),
                               reads=[Wk, hk], writes=["ps2"])
                    for tt in range(4):
                        t = stile * 4 + tt
                        op("act", lambda e, tt=tt, t=t: e.activation(out=va[:, t, 0:128], in_=ps[:, 2, tt * 128:(tt + 1) * 128],
                                                                     func=AF.Copy, scale=vtm[:, t:t + 1]),
                           reads=["ps2", "vtm"], writes=["va"])
                for m in range(4):
                    kbase = 4 * (12 + m)
                    nkt = kbase + 4
                    qs_sl = slice(m * 512, (m + 1) * 512)
                    def emit_score(kt, kbase=kbase, qs_sl=qs_sl):
                        d = kt - kbase
                        for c in range(2):
                            pb = 2 * c + (kt % 2)
                            Eb = E[c][kt % 2]
                            ek = "E%d_%d" % (c, kt % 2)
                            op("pe", lambda e, c=c, kt=kt, pb=pb, qs_sl=qs_sl: e.matmul(
                                ps[:, pb, :], lhsT=kT[c * 64:(c + 1) * 64, kt * 128:(kt + 1) * 128],
                                rhs=qT[c * 64:(c + 1) * 64, qs_sl], start=True, stop=True),
                               reads=["kT", "qT"], writes=["ps%d" % pb])
                            op("act", lambda e, Eb=Eb, pb=pb: e.activation(out=Eb[:], in_=ps[:, pb, :], func=AF.Exp, scale=0.125),
                               reads=["ps%d" % pb], writes=[ek])
                            if d >= 0:
                                op("pool", lambda e, Eb=Eb, d=d: e.tensor_tensor(out=Eb[:], in0=Eb[:], in1=cmb[:, d, :], op=ALU.mult),
                                   reads=[ek, "cmb"], writes=[ek])

                    def emit_pv(kt, kbase=kbase):
                        d = kt - kbase
                        for c in range(2):
                            Eb = E[c][kt % 2]
                            ek = "E%d_%d" % (c, kt % 2)
                            for qs in range(4):
                                if d > qs:
                                    continue
                                op("pe", lambda e, c=c, qs=qs, kt=kt, Eb=Eb, kbase=kbase: e.matmul(
                                    acc[c][:, qs, 0:129], lhsT=Eb[:, qs * 128:(qs + 1) * 128], rhs=va[:, kt, 0:129],
                                    start=(kt == 0), stop=(kt == kbase + qs)),
                                   reads=[ek, "va"], writes=[acck[c][qs // 2]])

                    for kt in range(nkt + 1):
                        if kt < nkt:
                            emit_score(kt)
                        if kt >= 1:
                            emit_pv(kt - 1)
                    for qs in range(4):
                        a0 = acc[0][:, qs, :]
                        a1 = acc[1][:, qs, :]
                        k0 = acck[0][qs // 2]
                        k1 = acck[1][qs // 2]
                        op("dve", lambda e, a0=a0: e.reciprocal(out=fs[:, 0:1], in_=a0[:, 128:129]), reads=[k0], writes=["fs"])
                        op("dve", lambda e, a1=a1: e.reciprocal(out=fs[:, 1:2], in_=a1[:, 128:129]), reads=[k1], writes=["fs"])
                        op("dve", lambda e: e.tensor_scalar(out=fs[:, 2:3], in0=fs[:, 1:2], scalar1=nlam, scalar2=None, op0=ALU.mult),
                           reads=["fs", "small"], writes=["fs"])
                        op("dve", lambda e, a0=a0: e.tensor_scalar(out=o0[:], in0=a0[:, 0:128], scalar1=fs[:, 0:1], scalar2=None, op0=ALU.mult),
                           reads=[k0, "fs"], writes=["o0"])
                        op("dve", lambda e, a1=a1: e.scalar_tensor_tensor(out=o1[:], in0=a1[:, 0:128], scalar=fs[:, 2:3], in1=o0[:],
                                                                          op0=ALU.mult, op1=ALU.add),
                           reads=[k1, "fs", "o0"], writes=["o1"])
                        op("act", lambda e: e.activation(out=ojunk[:], in_=o1[:], func=AF.Square, accum_out=fs[:, 3:4]),
                           reads=["o1"], writes=["ojunk", "fs"])
                        rstd_from_ss(fs[:, 3:4], 128, fs[:, 4:5], fs[:, 5:6], ["fs"])
                        op("dve", lambda e: e.scalar_tensor_tensor(out=o0[:], in0=o1[:], scalar=fs[:, 4:5], in1=hg[:], op0=ALU.mult, op1=ALU.mult),
                           reads=["o1", "fs", "hg"], writes=["o0"])
                        op("pe", lambda e, qs=qs: e.transpose(out=ps[:, 0, qs * 128:(qs + 1) * 128], in_=o0[:], identity=ident[:]),
                           reads=["o0", "ident"], writes=["ps0"])
                    op("act", lambda e: e.copy(out=yst[:], in_=ps[:, 0, :]), reads=["ps0"], writes=["yst"])
                    op("sp", lambda e, h=h, qs_sl=qs_sl: e.dma_start(out=yaT_d[:, h, qs_sl], in_=yst[:]), reads=["yst"], writes=["yaT_d"], dma=True)
        P.barrier()

        P.enabled = upto >= 3 and (only is None or only == 3)
        with contextlib.ExitStack() as st:
            wx = SB(st, "wx", [128, 8, 3072], BF16)
            wdt = SB(st, "wdt", [128, 8, 32], BF16)
            with contextlib.ExitStack() as st2:
                wst3 = [SB(st2, "wst3_%d" % i, [128, 8, 512]) for i in range(2)]
                for cb in range(6):
                    w = wst3[cb % 2]
                    wk = "wst3_%d" % (cb % 2)
                    op("sp", lambda e, w=w, cb=cb: e.dma_start(out=w[:], in_=wxbc_d[:, cb * 512:(cb + 1) * 512].rearrange("(kc p) n -> p kc n", p=128)),
                       writes=[wk], dma=True)
                    op("pool" if cb % 2 else "dve", lambda e, w=w, cb=cb: e.tensor_copy(out=wx[:, :, cb * 512:(cb + 1) * 512], in_=w[:]),
                       reads=[wk], writes=["wx"])
                w = wst3[0]
                op("sp", lambda e, w=w: e.dma_start(out=w[:, :, 0:32], in_=wdt_d.rearrange("(kc p) n -> p kc n", p=128)), writes=["wst3_0"], dma=True)
                op("dve", lambda e, w=w: e.tensor_copy(out=wdt[:], in_=w[:, :, 0:32]), reads=["wst3_0"], writes=["wdt"])
                P.barrier()
            cw = SB(st, "cw", [128, 24, 4])
            cbias = SB(st, "cbias", [128, 24])
            op("sp", lambda e: e.dma_start(out=cw[:], in_=convw_d), writes=["cw"], dma=True)
            op("sp", lambda e: e.dma_start(out=cbias[:], in_=convb_d), writes=["cbias"], dma=True)
            dtb = SB(st, "dtb", [128, 32])
            aneg = SB(st, "aneg", [128, 32])
            dsk = SB(st, "dsk", [128, 32])
            bcast_load(dtb[:], dtb_d, "dtb")
            bcast_load(aneg[:], alog_d, "aneg")
            bcast_load(dsk[:], dskip_d, "dsk")
            op("act", lambda e: e.activation(out=aneg[:], in_=aneg[:], func=AF.Exp), reads=["aneg"], writes=["aneg"])
            op("dve", lambda e: e.tensor_scalar(out=aneg[:], in0=aneg[:], scalar1=-1.0, scalar2=None, op0=ALU.mult), reads=["aneg"], writes=["aneg"])
            hsb = [SB(st, "h3_%d" % i, [128, 8, 512], BF16) for i in range(2)]
            pre = [SB(st, "pre%d" % i, [128, 515]) for i in range(4)]
            cv = [SB(st, "cv%d" % i, [128, 512]) for i in range(4)]
            slb = [SB(st, "sl%d" % i, [128, 512]) for i in range(4)]
            halo = SB(st, "halo", [128, 24, 3])
            x_tm = SB(st, "x_tm", [128, 4, 2048])
            xdt = SB(st, "xdt", [128, 4, 2048], BF16)
            xdw = SB(st, "xdw", [128, 2, 2048], BF16)
            xtmp = SB(st, "xtmp", [128, 2048])
            B_tm = SB(st, "B_tm", [128, 4, 512], BF16)
            B_cm = SB(st, "B_cm", [128, 4, 512], BF16)
            C_cm = SB(st, "C_cm", [128, 4, 512], BF16)
            ST = SB(st, "ST", [128, 2048])
            STb = SB(st, "STb", [128, 2048], BF16)
            dtr = SB(st, "dtr", [128, 4, 32])
            a_tm = SB(st, "a_tm", [128, 4, 32])
            acs4 = SB(st, "acs4", [128, 4, 64])
            wend4 = SB(st, "wend4", [128, 4, 32])
            etot4 = SB(st, "etot4", [128, 4, 32])
            eac4 = SB(st, "eac4", [128, 4, 32])
            dw4 = SB(st, "dw4", [128, 4, 32])
            cbm = SB(st, "cbm", [128, 128])
            arg = SB(st, "arg", [128, 128])
            dec = SB(st, "dec", [128, 128])
            arg8 = SB(st, "arg8", [128, 8, 128])
            dec8 = SB(st, "dec8", [128, 8, 128])
            MT8 = SB(st, "MT8", [128, 8, 128], BF16)
            ypre = SB(st, "ypre", [128, 2048])
            op("dve", lambda e: e.memset(halo[:], 0.0), writes=["halo"])
            op("dve", lambda e: e.memset(ST[:], 0.0), writes=["ST"])
            op("dve", lambda e: e.memset(STb[:], 0.0), writes=["STb"])

            def bc_h(ap32, nrep=64):
                return ap32.unsqueeze(2).to_broadcast([128, 32, nrep])

            cci = 0
            for stile in range(16):
                sl = slice(stile * 512, (stile + 1) * 512)
                hs = hsb[stile % 2]
                hk = "h3_%d" % (stile % 2)
                op("sp", lambda e, hs=hs, sl=sl: e.dma_start(out=hs[:], in_=hT_d[:, :, sl]), reads=["hT_d"], writes=[hk], dma=True)
                NBUF = 4
                SKEW = 3

                def stage_a(cc, bi, hs=hs, hk=hk):
                    pb = bi % 2
                    pr = pre[bi % NBUF]
                    prk = "pre%d" % (bi % NBUF)
                    cv_ = cv[bi % NBUF]
                    cvk = "cv%d" % (bi % NBUF)
                    sl_ = slb[bi % NBUF]
                    slk = "sl%d" % (bi % NBUF)
                    for kc in range(8):
                        op("pe", lambda e, kc=kc, cc=cc, hs=hs, pb=pb: e.matmul(ps[:, pb, :], lhsT=wx[:, kc, cc * 128:(cc + 1) * 128], rhs=hs[:, kc, :],
                                                                               start=(kc == 0), stop=(kc == 7)),
                           reads=["wx", hk], writes=["ps%d" % pb])
                    op("act", lambda e, pr=pr, pb=pb: e.copy(out=pr[:, 3:515], in_=ps[:, pb, :]),
                       reads=["ps%d" % pb], writes=[prk])
                    op("pool", lambda e, pr=pr, cc=cc: e.tensor_copy(out=pr[:, 0:3], in_=halo[:, cc, :]), reads=[("halo", cc)], writes=[prk])
                    op("pool", lambda e, pr=pr, cc=cc: e.tensor_copy(out=halo[:, cc, :], in_=pr[:, 512:515]), reads=[prk], writes=[("halo", cc)])
                    op("dve", lambda e, pr=pr, cv_=cv_, cc=cc: e.tensor_scalar(out=cv_[:], in0=pr[:, 0:512], scalar1=cw[:, cc, 0:1],
                                                                               scalar2=cbias[:, cc:cc + 1], op0=ALU.mult, op1=ALU.add),
                       reads=[prk, "cw", "cbias"], writes=[cvk])
                    for k in range(1, 4):
                        op("dve", lambda e, pr=pr, cv_=cv_, cc=cc, k=k: e.scalar_tensor_tensor(out=cv_[:], in0=pr[:, k:k + 512], scalar=cw[:, cc, k:k + 1],
                                                                                               in1=cv_[:], op0=ALU.mult, op1=ALU.add),
                           reads=[prk, "cw"], writes=[cvk])
                    if cc < 20:
                        op("act", lambda e, cv_=cv_, sl_=sl_: e.activation(out=sl_[:], in_=cv_[:], func=AF.Silu), reads=[cvk], writes=[slk])
                        if cc >= 16:
                            g = cc - 16
                            op("pool", lambda e, g=g, sl_=sl_: e.tensor_copy(out=B_cm[:, g, :], in_=sl_[:]), reads=[slk], writes=["B_cm"])
                    else:
                        g = cc - 20
                        op("act", lambda e, cv_=cv_, g=g: e.activation(out=C_cm[:, g, :], in_=cv_[:], func=AF.Silu), reads=[cvk], writes=["C_cm"])

                def stage_b(cc, bi):
                    if cc >= 20:
                        return
                    sl_ = slb[bi % NBUF]
                    slk = "sl%d" % (bi % NBUF)
                    for tt in range(4):
                        op("pe", lambda e, tt=tt, sl_=sl_: e.transpose(out=ps[:, 2, tt * 128:(tt + 1) * 128], in_=sl_[:, tt * 128:(tt + 1) * 128],
                                                                      identity=ident[:]),
                           reads=[slk, "ident"], writes=["ps2"])
                    src = ps[:, 2, :].rearrange("p (a b) -> p a b", b=128)
                    if cc < 16:
                        op("act", lambda e, cc=cc, src=src: e.copy(out=x_tm[:, :, cc * 128:(cc + 1) * 128], in_=src),
                           reads=["ps2"], writes=["x_tm"])
                    else:
                        g = cc - 16
                        op("act", lambda e, g=g, src=src: e.copy(out=B_tm[:, :, g * 128:(g + 1) * 128], in_=src),
                           reads=["ps2"], writes=["B_tm"])

                bis = {}
                for i in range(24 + SKEW):
                    if i < 24:
                        bis[i] = cci
                        stage_a(i, cci)
                        cci += 1
                    if i >= SKEW:
                        stage_b(i - SKEW, bis[i - SKEW])
                if upto == 3 and SUB < 2:
                    continue
                for tt in range(4):
                    for kc in range(8):
                        op("pe", lambda e, kc=kc, tt=tt, hs=hs: e.matmul(ps[:, 3, tt * 32:(tt + 1) * 32], lhsT=hs[:, kc, tt * 128:(tt + 1) * 128],
                                                                        rhs=wdt[:, kc, :], start=(kc == 0), stop=(kc == 7)),
                           reads=["wdt", hk], writes=["ps3"])
                op("dve", lambda e: e.tensor_tensor(out=dtr[:], in0=ps[:, 3, 0:128].rearrange("p (a b) -> p a b", b=32),
                                                    in1=dtb[:].unsqueeze(1).to_broadcast([128, 4, 32]), op=ALU.add),
                   reads=["ps3", "dtb"], writes=["dtr"])
                op("act", lambda e: e.activation(out=dtr[:], in_=dtr[:], func=AF.Exp), reads=["dtr"], writes=["dtr"])
                op("act", lambda e: e.activation(out=dtr[:], in_=dtr[:], func=AF.Ln, bias=1.0), reads=["dtr"], writes=["dtr"])
                for tt in range(4):
                    t = stile * 4 + tt
                    op("dve", lambda e, tt=tt, t=t: e.tensor_scalar(out=dtr[:, tt, :], in0=dtr[:, tt, :], scalar1=vtm[:, t:t + 1], scalar2=None, op0=ALU.mult),
                       reads=["dtr", "vtm"], writes=["dtr"])
                op("dve", lambda e: e.tensor_tensor(out=a_tm[:], in0=dtr[:], in1=aneg[:].unsqueeze(1).to_broadcast([128, 4, 32]), op=ALU.mult),
                   reads=["dtr", "aneg"], writes=["a_tm"])
                own = stile >= 12 and not (upto == 3 and SUB < 4)
                HL = not (upto == 3 and SUB < 5)
                if upto == 3 and SUB < 3:
                    continue
                for tt in range(4):
                    c0 = 128 + tt * 64
                    op("pe", lambda e, tt=tt, c0=c0: e.matmul(ps[:, 3, c0:c0 + 32], lhsT=tri[:], rhs=a_tm[:, tt, :], start=True, stop=True),
                       reads=["tri", "a_tm"], writes=["ps3"])
                    op("pe", lambda e, tt=tt, c0=c0: e.matmul(ps[:, 3, c0 + 32:c0 + 64], lhsT=ones[:], rhs=a_tm[:, tt, :], start=True, stop=True),
                       reads=["ones", "a_tm"], writes=["ps3"])
                op("dve", lambda e: e.tensor_copy(out=acs4[:], in_=ps[:, 3, 128:384].rearrange("p (a b) -> p a b", b=64)), reads=["ps3"], writes=["acs"])
                op("dve", lambda e: e.tensor_tensor(out=wend4[:], in0=acs4[:, :, 32:64], in1=acs4[:, :, 0:32], op=ALU.subtract), reads=["acs"], writes=["wend"])
                op("act", lambda e: e.activation(out=wend4[:], in_=wend4[:], func=AF.Exp), reads=["wend"], writes=["wend"])
                op("act", lambda e: e.activation(out=etot4[:], in_=acs4[:, :, 32:64], func=AF.Exp), reads=["acs"], writes=["etot"])
                if own:
                    op("act", lambda e: e.activation(out=eac4[:], in_=acs4[:, :, 0:32], func=AF.Exp), reads=["acs"], writes=["eac"])
                op("dve", lambda e: e.tensor_tensor(out=dw4[:], in0=dtr[:], in1=wend4[:], op=ALU.mult), reads=["dtr", "wend"], writes=["dw4"])
                for tt in range(4):
                    op("dve", lambda e, tt=tt: e.tensor_tensor(out=xdt[:, tt, :].rearrange("p (h d) -> p h d", d=64),
                                                               in0=x_tm[:, tt, :].rearrange("p (h d) -> p h d", d=64),
                                                               in1=bc_h(dtr[:, tt, :]), op=ALU.mult),
                       reads=["x_tm", "dtr"], writes=[("xdt", tt)])
                for tt in range(4):
                    op("dve", lambda e, tt=tt: e.tensor_tensor(out=xdw[:, tt % 2, :].rearrange("p (h d) -> p h d", d=64),
                                                               in0=x_tm[:, tt, :].rearrange("p (h d) -> p h d", d=64),
                                                               in1=bc_h(dw4[:, tt, :]), op=ALU.mult),
                       reads=["x_tm", "dw4"], writes=[("xdw", tt % 2)])
                    if own:
                        tsl = slice(tt * 128, (tt + 1) * 128)
                        for g in range(4):
                            op("pe", lambda e, g=g, tsl=tsl: e.matmul(ps[:, 7, 0:128], lhsT=B_cm[:, g, tsl], rhs=C_cm[:, g, tsl], start=True, stop=True),
                               reads=["B_cm", "C_cm"], writes=["ps7"])
                            op("dve", lambda e: e.tensor_tensor(out=cbm[:], in0=ps[:, 7, 0:128], in1=tri[:], op=ALU.mult),
                               reads=["ps7", "tri"], writes=["cbm"])
                            op("pe", lambda e, g=g, tsl=tsl: e.matmul(ps[:, 4, :], lhsT=C_cm[:, g, tsl], rhs=STb[:, g * 512:(g + 1) * 512], start=True, stop=True),
                               reads=["C_cm", "STb"], writes=["ps4"])
                            for hh in range(8):
                                hd = g * 8 + hh
                                rb_ = 6 + hh // 4
                                op("pe", lambda e, tt=tt, hd=hd, hh=hh, rb_=rb_: e.matmul(ps[:, rb_, (hh % 4) * 128:(hh % 4 + 1) * 128],
                                                                                      lhsT=a_tm[:, tt, hd:hd + 1].to_broadcast([128, 128]),
                                                                                      rhs=tri[:], start=True, stop=True),
                                   reads=["a_tm", "tri"], writes=["ps%d" % rb_])
                            for hh in range(8):
                                hd = g * 8 + hh
                                rb_ = 6 + hh // 4
                                op("dve", lambda e, hd=hd, hh=hh, rb_=rb_, tt=tt: e.tensor_scalar(out=arg8[:, hh, :], in0=ps[:, rb_, (hh % 4) * 128:(hh % 4 + 1) * 128],
                                                                                       scalar1=acs4[:, tt, hd:hd + 1], scalar2=0.0, op0=ALU.subtract, op1=ALU.min),
                                   reads=["ps%d" % rb_, "acs"], writes=["arg8"])
                            op("act", lambda e: e.activation(out=dec8[:], in_=arg8[:], func=AF.Exp), reads=["arg8"], writes=["dec8"])
                            op("dve", lambda e: e.tensor_tensor(out=MT8[:], in0=dec8[:], in1=cbm[:].unsqueeze(1).to_broadcast([128, 8, 128]), op=ALU.mult),
                               reads=["dec8", "cbm"], writes=["MT8"])
                            for hh in range(8):
                                hd = g * 8 + hh
                                op("pe", lambda e, hh=hh, hd=hd, tt=tt: e.matmul(ps[:, 5, hh * 64:(hh + 1) * 64], lhsT=MT8[:, hh, :],
                                                                               rhs=xdt[:, tt, hd * 64:(hd + 1) * 64], start=True, stop=True),
                                   reads=["MT8", ("xdt", tt)], writes=["ps5"])
                            gsl = slice(g * 512, (g + 1) * 512)
                            op("dve", lambda e, g=g, gsl=gsl, tt=tt: e.tensor_tensor(out=ypre[:, gsl].rearrange("p (h d) -> p h d", d=64),
                                                                              in0=ps[:, 4, :].rearrange("p (h d) -> p h d", d=64),
                                                                              in1=eac4[:, tt, g * 8:(g + 1) * 8].unsqueeze(2).to_broadcast([128, 8, 64]), op=ALU.mult),
                               reads=["ps4", "eac"], writes=["ypre"])
                            if HL:
                                op("dve", lambda e, gsl=gsl: e.tensor_tensor(out=ypre[:, gsl], in0=ypre[:, gsl], in1=ps[:, 5, :], op=ALU.add),
                                   reads=["ps5", "ypre"], writes=["ypre"])
                        op("dve", lambda e, tt=tt: e.tensor_tensor(out=xtmp[:].rearrange("p (h d) -> p h d", d=64),
                                                                   in0=x_tm[:, tt, :].rearrange("p (h d) -> p h d", d=64),
                                                                   in1=bc_h(dsk[:]), op=ALU.mult),
                           reads=["x_tm", "dsk"], writes=["xtmp"])
                        op("dve", lambda e: e.tensor_tensor(out=ypre[:], in0=ypre[:], in1=xtmp[:], op=ALU.add), reads=["xtmp", "ypre"], writes=["ypre"])
                        r0 = (stile - 12) * 512 + tt * 128
                        op("sp", lambda e, r0=r0: e.dma_start(out=ypre_d[r0:r0 + 128, :], in_=ypre[:]), reads=["ypre"], writes=["ypre_d"], dma=True)
                    for g in range(4):
                        pbk = 4 + g
                        gsl = slice(g * 512, (g + 1) * 512)
                        op("pe", lambda e, g=g, tt=tt, pbk=pbk, gsl=gsl: e.matmul(ps[:, pbk, :], lhsT=B_tm[:, tt, g * 128:(g + 1) * 128], rhs=xdw[:, tt % 2, gsl],
                                                                                 start=True, stop=True),
                           reads=["B_tm", ("xdw", tt % 2)], writes=["ps%d" % pbk])
                        op("dve", lambda e, g=g, gsl=gsl, tt=tt: e.tensor_tensor(out=ST[:, gsl].rearrange("p (h d) -> p h d", d=64),
                                                                          in0=ST[:, gsl].rearrange("p (h d) -> p h d", d=64),
                                                                          in1=etot4[:, tt, g * 8:(g + 1) * 8].unsqueeze(2).to_broadcast([128, 8, 64]), op=ALU.mult),
                           reads=["ST", "etot"], writes=["ST"])
                        op("dve", lambda e, pbk=pbk, gsl=gsl: e.tensor_tensor(out=ST[:, gsl], in0=ST[:, gsl], in1=ps[:, pbk, :], op=ALU.add),
                           reads=["ST", "ps%d" % pbk], writes=["ST"])
                    if stile >= 11:
                        op("act", lambda e: e.copy(out=STb[:], in_=ST[:]), reads=["ST"], writes=["STb"])
        P.barrier()

        P.enabled = upto >= 4 and (only is None or only == 4)
        def load_w_bf16(st, name, src, kchunks, ncols, engs=("dve", "pool")):
            wt = SB(st, name, [128, kchunks, ncols], BF16)
            with contextlib.ExitStack() as st2:
                stg = [SB(st2, name + "_s%d" % i, [128, 8, 512]) for i in range(2)]
                i = 0
                for k0 in range(0, kchunks, 8):
                    for cb in range(ncols // 512):
                        s_ = stg[i % 2]
                        sk_ = name + "_s%d" % (i % 2)
                        op("sp", lambda e, s_=s_, k0=k0, cb=cb: e.dma_start(
                            out=s_[:], in_=src[k0 * 128:(k0 + 8) * 128, cb * 512:(cb + 1) * 512].rearrange("(kc p) n -> p kc n", p=128)),
                           writes=[sk_], dma=True)
                        op(engs[i % 2], lambda e, s_=s_, k0=k0, cb=cb: e.tensor_copy(out=wt[:, k0:k0 + 8, cb * 512:(cb + 1) * 512], in_=s_[:]),
                           reads=[sk_], writes=[name])
                        i += 1
                P.barrier()
            return wt

        with contextlib.ExitStack() as st:
            wz = load_w_bf16(st, "wz", wz_d, 8, 2048)
            wbs = load_w_bf16(st, "wbs", wbs_d, 16, 1024)
            ssn = SB(st, "ssn", [128, 2048])
            bcast_load(ssn[:], ssdn_d, "ssn")
            hto = [SB(st, "hto%d" % i, [128, 8, 128], BF16) for i in range(2)]
            ypt = [SB(st, "ypt%d" % i, [128, 2048]) for i in range(2)]
            zs = SB(st, "zs", [128, 512])
            yg = SB(st, "yg", [128, 512])
            yjunk = SB(st, "yjunk", [128, 512], BF16)
            ysT = SB(st, "ysT", [128, 16, 128], BF16)
            psb = SB(st, "psb", [128, 1024])
            f4 = SB(st, "f4", [128, 4])
            for t in range(16):
                ht = hto[t % 2]
                htk = "hto%d" % (t % 2)
                yp = ypt[t % 2]
                ypk = "ypt%d" % (t % 2)
                c0 = 6144 + t * 128
                op("sp", lambda e, ht=ht, c0=c0: e.dma_start(out=ht[:], in_=hT_d[:, :, c0:c0 + 128]), reads=["hT_d"], writes=[htk], dma=True)
                op("sp", lambda e, yp=yp, t=t: e.dma_start(out=yp[:], in_=ypre_d[t * 128:(t + 1) * 128, :]), reads=["ypre_d"], writes=[ypk], dma=True)
                for cb in range(4):
                    csl = slice(cb * 512, (cb + 1) * 512)
                    pb = cb % 2
                    for kc in range(8):
                        op("pe", lambda e, kc=kc, ht=ht, csl=csl, pb=pb: e.matmul(ps[:, pb, :], lhsT=ht[:, kc, :], rhs=wz[:, kc, csl],
                                                                                 start=(kc == 0), stop=(kc == 7)),
                           reads=[htk, "wz"], writes=["ps%d" % pb])
                    op("act", lambda e, pb=pb: e.activation(out=zs[:], in_=ps[:, pb, :], func=AF.Silu), reads=["ps%d" % pb], writes=["zs"])
                    op("dve", lambda e, yp=yp, csl=csl: e.tensor_tensor(out=yg[:], in0=yp[:, csl], in1=zs[:], op=ALU.mult), reads=[ypk, "zs"], writes=["yg"])
                    op("act", lambda e: e.activation(out=yjunk[:], in_=yg[:], func=AF.Square, accum_out=f4[:, 0:1]), reads=["yg"], writes=["yjunk", "f4"])
                    rstd_from_ss(f4[:, 0:1], 512, f4[:, 1:2], f4[:, 2:3], ["f4"])
                    op("dve", lambda e, csl=csl: e.scalar_tensor_tensor(out=yg[:], in0=yg[:], scalar=f4[:, 1:2], in1=ssn[:, csl], op0=ALU.mult, op1=ALU.mult),
                       reads=["yg", "f4", "ssn"], writes=["yg"])
                    for i in range(4):
                        op("pe", lambda e, i=i: e.transpose(out=ps[:, 2, i * 128:(i + 1) * 128], in_=yg[:, i * 128:(i + 1) * 128], identity=ident[:]),
                           reads=["yg", "ident"], writes=["ps2"])
                    op("act", lambda e, cb=cb: e.copy(out=ysT[:, cb * 4:(cb + 1) * 4, :], in_=ps[:, 2, :].rearrange("p (a b) -> p a b", b=128)),
                       reads=["ps2"], writes=["ysT"])
                for half in range(2):
                    for kc in range(16):
                        op("pe", lambda e, kc=kc, half=half: e.matmul(ps[:, 4 + half, :], lhsT=ysT[:, kc, :], rhs=wbs[:, kc, half * 512:(half + 1) * 512],
                                                                     start=(kc == 0), stop=(kc == 15)),
                           reads=["ysT", "wbs"], writes=["ps%d" % (4 + half)])
                op("dve", lambda e: e.tensor_copy(out=psb[:].rearrange("p (a b) -> p a b", b=512), in_=ps[:, 4:6, :]), reads=["ps4", "ps5"], writes=["psb"])
                op("sp", lambda e, t=t: e.dma_start(out=ps_d[t * 128:(t + 1) * 128, :], in_=psb[:]), reads=["psb"], writes=["ps_d"], dma=True)
        P.barrier()

        P.enabled = upto >= 5 and (only is None or only == 5)
        with contextlib.ExitStack() as stB:
            h2T = SB(stB, "h2T", [128, 8, 2048], BF16)
            G = SB(stB, "G", [128, 16, 257])
            for t_ in range(16):
                op("dve", lambda e, t_=t_: e.memset(G[:, t_, :], 1.0), writes=["G"])
            with contextlib.ExitStack() as st:
                wg = load_w_bf16(st, "wg", wg_d, 8, 2048)
                wba = load_w_bf16(st, "wba", wba_d, 8, 1024)
                wo = load_w_bf16(st, "wo", wo_d, 8, 1024)
                wr = SB(st, "wr", [128, 8, 256])
                op("sp", lambda e: e.dma_start(out=wr[:], in_=wr_d.rearrange("(kc p) n -> p kc n", p=128)), writes=["wr"], dma=True)
                bg = SB(st, "bg", [128, 2048])
                bcast_load(bg[:], b_gate, "bg")
                gm_ = SB(st, "gm_", [128, 1024])
                mod_bc(gm_[:], 2, "gm_")
                shf = SB(st, "shf", [128, 1024])
                mod_bc(shf[:], 3, "shf")
                g2 = SB(st, "g2", [128, 1024])
                mod_bc(g2[:], 4, "g2")
                h2 = SB(st, "h2", [128, 1024])
                bcast_load(h2[:], norm_ffn, "h2")
                op("dve", lambda e: e.scalar_tensor_tensor(out=g2[:], in0=g2[:], scalar=1.0, in1=h2[:], op0=ALU.add, op1=ALU.mult),
                   reads=["g2", "h2"], writes=["g2"])
                rb = SB(st, "rb", [128, 256])
                bcast_load(rb[:], rbias_d, "rb")
                hto = [SB(st, "htb%d" % i, [128, 8, 128], BF16) for i in range(2)]
                yat = [SB(st, "yat%d" % i, [128, 8, 128], BF16) for i in range(2)]
                pst = [SB(st, "pst%d" % i, [128, 1024]) for i in range(2)]
                xo = [SB(st, "xo%d" % i, [128, 1024]) for i in range(2)]
                gt = SB(st, "gt", [128, 2048])
                mixed = SB(st, "mixed", [128, 1024])
                mT = SB(st, "mT", [128, 8, 128], BF16)
                x1 = SB(st, "x1", [128, 1024])
                hjunk = SB(st, "hjunk", [128, 1024], BF16)
                h2Tf = SB(st, "h2Tf", [128, 8, 128])
                scr = SB(st, "scr", [128, 256])
                cho = SB(st, "cho", [128, 256])
                msk = SB(st, "msk", [128, 256])
                m8 = SB(st, "m8", [128, 8, 8])
                gs = SB(st, "gs", [128, 8])
                t8 = SB(st, "t8", [128, 8])
                gmk = SB(st, "gmk", [128, 8])
                pen = SB(st, "pen", [128, 8])
                m8b = SB(st, "m8b", [128, 8])
                f5 = SB(st, "f5", [128, 8])
                for t in range(16):
                    ht = hto[t % 2]
                    htk = "htb%d" % (t % 2)
                    ya = yat[t % 2]
                    yak = "yat%d" % (t % 2)
                    pt = pst[t % 2]
                    ptk = "pst%d" % (t % 2)
                    xo_ = xo[t % 2]
                    xok = "xo%d" % (t % 2)
                    c0 = 6144 + t * 128
                    tsl = slice(t * 128, (t + 1) * 128)
                    op("sp", lambda e, ht=ht, c0=c0: e.dma_start(out=ht[:], in_=hT_d[:, :, c0:c0 + 128]), reads=["hT_d"], writes=[htk], dma=True)
                    op("sp", lambda e, ya=ya, tsl=tsl: e.dma_start(out=ya[:], in_=yaT_d[:, :, tsl]), reads=["yaT_d"], writes=[yak], dma=True)
                    op("sp", lambda e, pt=pt, tsl=tsl: e.dma_start(out=pt[:], in_=ps_d[tsl, :]), reads=["ps_d"], writes=[ptk], dma=True)
                    op("sp", lambda e, xo_=xo_, c0=c0: e.dma_start(out=xo_[:], in_=xl[c0:c0 + 128, :]), writes=[xok], dma=True)
                    for cb in range(4):
                        csl = slice(cb * 512, (cb + 1) * 512)
                        pb = cb % 2
                        for kc in range(8):
                            op("pe", lambda e, kc=kc, ht=ht, csl=csl, pb=pb: e.matmul(ps[:, pb, :], lhsT=ht[:, kc, :], rhs=wg[:, kc, csl],
                                                                                     start=(kc == 0), stop=(kc == 7)),
                               reads=[htk, "wg"], writes=["ps%d" % pb])
                        op("dve", lambda e, csl=csl, pb=pb: e.tensor_tensor(out=gt[:, csl], in0=ps[:, pb, :], in1=bg[:, csl], op=ALU.add),
                           reads=["ps%d" % pb, "bg"], writes=["gt"])
                    op("act", lambda e: e.activation(out=gt[:], in_=gt[:], func=AF.Sigmoid), reads=["gt"], writes=["gt"])
                    if upto == 5 and SUB < 3:
                        continue
                    for half in range(2):
                        hsl = slice(half * 512, (half + 1) * 512)
                        for kc in range(8):
                            op("pe", lambda e, kc=kc, ya=ya, hsl=hsl, half=half: e.matmul(ps[:, 2 + half, :], lhsT=ya[:, kc, :], rhs=wba[:, kc, hsl],
                                                                                         start=(kc == 0), stop=(kc == 7)),
                               reads=[yak, "wba"], writes=["ps%d" % (2 + half)])
                        op("dve", lambda e, hsl=hsl, half=half: e.tensor_tensor(out=mixed[:, hsl], in0=ps[:, 2 + half, :], in1=gt[:, hsl], op=ALU.mult),
                           reads=["ps%d" % (2 + half), "gt"], writes=["mixed"])
                    op("dve", lambda e, pt=pt: e.tensor_tensor(out=h2[:], in0=pt[:], in1=gt[:, 1024:2048], op=ALU.mult), reads=[ptk, "gt"], writes=["h2"])
                    op("dve", lambda e: e.tensor_tensor(out=mixed[:], in0=mixed[:], in1=h2[:], op=ALU.add), reads=["mixed", "h2"], writes=["mixed"])
                    if upto == 5 and SUB < 4:
                        continue
                    for kc in range(8):
                        bk = 4 + kc // 4
                        op("pe", lambda e, kc=kc, bk=bk: e.transpose(out=ps[:, bk, (kc % 4) * 128:(kc % 4 + 1) * 128], in_=mixed[:, kc * 128:(kc + 1) * 128],
                                                                    identity=ident[:]),
                           reads=["mixed", "ident"], writes=["ps%d" % bk])
                    op("act", lambda e: e.copy(out=mT[:], in_=ps[:, 4:6, :].rearrange("p a (b c) -> p (a b) c", c=128)), reads=["ps4", "ps5"], writes=["mT"])
                    for half in range(2):
                        hsl = slice(half * 512, (half + 1) * 512)
                        for kc in range(8):
                            op("pe", lambda e, kc=kc, hsl=hsl, half=half: e.matmul(ps[:, 6 + half, :], lhsT=mT[:, kc, :], rhs=wo[:, kc, hsl],
                                                                                  start=(kc == 0), stop=(kc == 7)),
                               reads=["mT", "wo"], writes=["ps%d" % (6 + half)])
                        op("dve", lambda e, hsl=hsl, half=half: e.tensor_tensor(out=x1[:, hsl], in0=ps[:, 6 + half, :], in1=gm_[:, hsl], op=ALU.mult),
                           reads=["ps%d" % (6 + half), "gm_"], writes=["x1"])
                    op("dve", lambda e, xo_=xo_: e.tensor_tensor(out=x1[:], in0=x1[:], in1=xo_[:], op=ALU.add), reads=["x1", xok], writes=["x1"])
                    op("sp", lambda e, tsl=tsl: e.dma_start(out=x1_d[tsl, :], in_=x1[:]), reads=["x1"], writes=["x1_d"], dma=True)
                    if upto == 5 and SUB < 5:
                        continue
                    op("act", lambda e: e.activation(out=hjunk[:], in_=x1[:], func=AF.Square, accum_out=f5[:, 0:1]), reads=["x1"], writes=["hjunk", "f5"])
                    rstd_from_ss(f5[:, 0:1], 1024, f5[:, 1:2], f5[:, 2:3], ["f5"])
                    op("dve", lambda e: e.scalar_tensor_tensor(out=h2[:], in0=x1[:], scalar=f5[:, 1:2], in1=g2[:], op0=ALU.mult, op1=ALU.mult),
                       reads=["x1", "f5", "g2"], writes=["h2"])
                    op("dve", lambda e: e.tensor_tensor(out=h2[:], in0=h2[:], in1=shf[:], op=ALU.add), reads=["h2", "shf"], writes=["h2"])
                    for kc in range(8):
                        bk = 4 + kc // 4
                        op("pe", lambda e, kc=kc, bk=bk: e.transpose(out=ps[:, bk, (kc % 4) * 128:(kc % 4 + 1) * 128], in_=h2[:, kc * 128:(kc + 1) * 128],
                                                                    identity=ident[:]),
                           reads=["h2", "ident"], writes=["ps%d" % bk])
                    srcT = ps[:, 4:6, :].rearrange("p a (b c) -> p (a b) c", c=128)
                    op("act", lambda e, srcT=srcT: e.copy(out=h2Tf[:], in_=srcT), reads=["ps4", "ps5"], writes=["h2Tf"])
                    op("dve", lambda e, tsl=tsl: e.tensor_copy(out=h2T[:, :, tsl], in_=h2Tf[:]), reads=["h2Tf"], writes=["h2T"])
                    if upto == 5 and SUB < 6:
                        continue
                    for kc in range(8):
                        op("pe", lambda e, kc=kc: e.matmul(ps[:, 0, 0:256], lhsT=h2Tf[:, kc, :], rhs=wr[:, kc, :], start=(kc == 0), stop=(kc == 7)),
                           reads=["h2Tf", "wr"], writes=["ps0"])
                    op("act", lambda e: e.activation(out=scr[:], in_=ps[:, 0, 0:256], func=AF.Sigmoid), reads=["ps0"], writes=["scr"])
                    op("dve", lambda e: e.tensor_tensor(out=cho[:], in0=scr[:], in1=rb[:], op=ALU.add), reads=["scr", "rb"], writes=["cho"])
                    for g in range(8):
                        op("dve", lambda e, g=g: e.max(out=m8[:, g, :], in_=cho[:, g * 32:(g + 1) * 32]), reads=["cho"], writes=["m8"])
                    op("dve", lambda e: e.tensor_tensor(out=gs[:], in0=m8[:, :, 0], in1=m8[:, :, 1], op=ALU.add), reads=["m8"], writes=["gs"])
                    op("dve", lambda e: e.max(out=t8[:], in_=gs[:]), reads=["gs"], writes=["t8"])
                    op("dve", lambda e: e.tensor_scalar(out=gmk[:], in0=gs[:], scalar1=t8[:, 3:4], scalar2=None, op0=ALU.is_ge), reads=["gs", "t8"], writes=["gmk"])
                    op("dve", lambda e: e.tensor_scalar(out=pen[:], in0=gmk[:], scalar1=1.0, scalar2=1e30, op0=ALU.subtract, op1=ALU.mult),
                       reads=["gmk"], writes=["pen"])
                    op("dve", lambda e: e.tensor_tensor(out=msk[:].rearrange("p (g k) -> p g k", k=32), in0=cho[:].rearrange("p (g k) -> p g k", k=32),
                                                        in1=gmk[:].unsqueeze(2).to_broadcast([128, 8, 32]), op=ALU.mult), reads=["cho", "gmk"], writes=["msk"])
                    op("dve", lambda e: e.tensor_tensor(out=msk[:].rearrange("p (g k) -> p g k", k=32), in0=msk[:].rearrange("p (g k) -> p g k", k=32),
                                                        in1=pen[:].unsqueeze(2).to_broadcast([128, 8, 32]), op=ALU.add), reads=["msk", "pen"], writes=["msk"])
                    op("dve", lambda e: e.max(out=m8b[:], in_=msk[:]), reads=["msk"], writes=["m8b"])
                    op("dve", lambda e: e.tensor_scalar(out=msk[:], in0=msk[:], scalar1=m8b[:, 7:8], scalar2=None, op0=ALU.is_ge), reads=["msk", "m8b"], writes=["msk"])
                    op("dve", lambda e: e.tensor_tensor(out=msk[:], in0=msk[:], in1=scr[:], op=ALU.mult), reads=["msk", "scr"], writes=["msk"])
                    op("dve", lambda e: e.reduce_sum(out=f5[:, 3:4], in_=msk[:], axis=AX.X), reads=["msk"], writes=["f5"])
                    op("dve", lambda e: e.tensor_scalar(out=f5[:, 3:4], in0=f5[:, 3:4], scalar1=1e-20, scalar2=None, op0=ALU.add), reads=["f5"], writes=["f5"])
                    op("dve", lambda e: e.reciprocal(out=f5[:, 4:5], in_=f5[:, 3:4]), reads=["f5"], writes=["f5"])
                    op("dve", lambda e, t=t: e.tensor_scalar(out=G[:, t, 0:256], in0=msk[:], scalar1=f5[:, 4:5], scalar2=2.5, op0=ALU.mult, op1=ALU.mult),
                       reads=["msk", "f5"], writes=["G"])
            P.barrier()
            if debug:
                dG = nc.dram_tensor("dbg_G", [128, 16, 257], F32, kind="ExternalOutput").ap()
                dH = nc.dram_tensor("dbg_h2T", [128, 8, 2048], BF16, kind="ExternalOutput").ap()
                for t_ in range(16):
                    op("sp", lambda e, t_=t_: e.dma_start(out=dG[:, t_, :], in_=G[:, t_, :]), reads=["G"], dma=True, is_out=True)
                for k_ in range(8):
                    op("sp", lambda e, k_=k_: e.dma_start(out=dH[:, k_, :], in_=h2T[:, k_, :]), reads=["h2T"], dma=True, is_out=True)

            P.enabled = upto >= 6 and (only is None or only == 6)
            accm = SB(stB, "accm", [128, 16, 1024])
            for t_ in range(16):
                op("pool", lambda e, t_=t_: e.memset(accm[:, t_, :], 0.0), writes=[("acc", t_)])
            with contextlib.ExitStack() as st:
                sg_f = [SB(st, "sgf0", [128, 8, 256])] * 2
                su_f = [SB(st, "suf0", [128, 8, 256])] * 2
                sd_f = [SB(st, "sdf0", [128, 2, 1024])] * 2
                wgb = [SB(st, "wgb%d" % i, [128, 8, 256], BF16) for i in range(2)]
                wub = [SB(st, "wub%d" % i, [128, 8, 256], BF16) for i in range(2)]
                wdb = [SB(st, "wdb%d" % i, [128, 2, 1024], BF16) for i in range(2)]
                sgt = [SB(st, "sgt%d" % i, [128, 1024]) for i in range(2)]
                actb = [SB(st, "actb%d" % i, [128, 2, 512], BF16) for i in range(2)]
                di = [0]

                def emit_load(ex):
                    b = ex % 2
                    op("sp", lambda e, ex=ex, b=b: e.dma_start(out=sg_f[b][:], in_=weg[ex].rearrange("(kc p) n -> p kc n", p=128)), writes=["sgf0"], dma=True)
                    op("sp", lambda e, ex=ex, b=b: e.dma_start(out=su_f[b][:], in_=weu[ex].rearrange("(kc p) n -> p kc n", p=128)), writes=["suf0"], dma=True)
                    op("sp", lambda e, ex=ex, b=b: e.dma_start(out=sd_f[b][:], in_=wed[ex].rearrange("(kc p) n -> p kc n", p=128)), writes=["sdf0"], dma=True)
                    op("pool", lambda e, b=b: e.tensor_copy(out=wgb[b][:], in_=sg_f[b][:]), reads=["sgf0"], writes=["wgb%d" % b])
                    op("pool", lambda e, b=b: e.tensor_copy(out=wub[b][:], in_=su_f[b][:]), reads=["suf0"], writes=["wub%d" % b])
                    op("pool", lambda e, b=b: e.tensor_copy(out=wdb[b][:], in_=sd_f[b][:]), reads=["sdf0"], writes=["wdb%d" % b])

                def emit_gu(ex, tq, ab):
                    b = ex % 2
                    qsl = slice(tq * 512, (tq + 1) * 512)
                    for fb in range(2):
                        for kc in range(8):
                            op("pe", lambda e, kc=kc, fb=fb, b=b, qsl=qsl: e.matmul(ps[:, fb, :], lhsT=wgb[b][:, kc, fb * 128:(fb + 1) * 128], rhs=h2T[:, kc, qsl],
                                                                                   start=(kc == 0), stop=(kc == 7)),
                               reads=["wgb%d" % b, "h2T"], writes=["ps%d" % fb])
                    for fb in range(2):
                        for kc in range(8):
                            op("pe", lambda e, kc=kc, fb=fb, b=b, qsl=qsl: e.matmul(ps[:, 2 + fb, :], lhsT=wub[b][:, kc, fb * 128:(fb + 1) * 128], rhs=h2T[:, kc, qsl],
                                                                                   start=(kc == 0), stop=(kc == 7)),
                               reads=["wub%d" % b, "h2T"], writes=["ps%d" % (2 + fb)])
                    op("act", lambda e, ab=ab: e.activation(out=sgt[ab][:].rearrange("p (a b) -> p a b", b=512), in_=ps[:, 0:2, :], func=AF.Silu),
                       reads=["ps0", "ps1"], writes=["sgt%d" % ab])
                    op("dve", lambda e, ab=ab: e.tensor_tensor(out=actb[ab][:], in0=sgt[ab][:].rearrange("p (a b) -> p a b", b=512), in1=ps[:, 2:4, :], op=ALU.mult),
                       reads=["sgt%d" % ab, "ps2", "ps3"], writes=["actb%d" % ab])

                def emit_down(ex, tq, ab):
                    b = ex % 2
                    gcol = ex if ex < n_exp else 256
                    for tt in range(4):
                        t = tq * 4 + tt
                        pd = 4 + 2 * (di[0] % 2)
                        di[0] += 1
                        for half in range(2):
                            for fb in range(2):
                                op("pe", lambda e, fb=fb, half=half, tt=tt, ab=ab, b=b, pd=pd: e.matmul(
                                    ps[:, pd + half, :], lhsT=actb[ab][:, fb, tt * 128:(tt + 1) * 128], rhs=wdb[b][:, fb, half * 512:(half + 1) * 512],
                                    start=(fb == 0), stop=(fb == 1)),
                                   reads=["actb%d" % ab, "wdb%d" % b], writes=["ps%d" % (pd + half)])
                        op("dve", lambda e, t=t, gcol=gcol, pd=pd: e.scalar_tensor_tensor(
                            out=accm[:, t, :].rearrange("p (a b) -> p a b", b=512), in0=ps[:, pd:pd + 2, :], scalar=G[:, t, gcol:gcol + 1],
                            in1=accm[:, t, :].rearrange("p (a b) -> p a b", b=512), op0=ALU.mult, op1=ALU.add),
                           reads=["ps%d" % pd, "ps%d" % (pd + 1), "G", ("acc", t)], writes=[("acc", t)])

                units = [(ex, tq) for ex in range(n_exp + 1) for tq in range(4)]
                prev = None
                for ui, (ex, tq) in enumerate(units):
                    if tq == 0:
                        emit_load(ex)
                    ab = ui % 2
                    emit_gu(ex, tq, ab)
                    if prev is not None:
                        emit_down(*prev)
                    prev = (ex, tq, ab)
                emit_down(*prev)
                P.barrier()
            P.enabled = upto >= 7 and (only is None or only == 7)
            with contextlib.ExitStack() as st:
                gf = SB(st, "gf", [128, 1024])
                mod_bc(gf[:], 5, "gf")
                nfin = SB(st, "nfin", [128, 1024])
                bcast_load(nfin[:], norm_final, "nfin")
                x1b = [SB(st, "x1b%d" % i, [128, 1024]) for i in range(2)]
                ob = [SB(st, "ob%d" % i, [128, 1024]) for i in range(2)]
                fj = SB(st, "fj", [128, 1024], BF16)
                f6 = SB(st, "f6", [128, 4])
                for t in range(16):
                    tsl = slice(t * 128, (t + 1) * 128)
                    xb_ = x1b[t % 2]
                    xbk = "x1b%d" % (t % 2)
                    o_ = ob[t % 2]
                    obk = "ob%d" % (t % 2)
                    op("sp", lambda e, xb_=xb_, tsl=tsl: e.dma_start(out=xb_[:], in_=x1_d[tsl, :]), reads=["x1_d"], writes=[xbk], dma=True)
                    op("dve", lambda e, t=t, o_=o_: e.tensor_tensor(out=o_[:], in0=accm[:, t, :], in1=gf[:], op=ALU.mult), reads=[("acc", t), "gf"], writes=[obk])
                    op("dve", lambda e, o_=o_, xb_=xb_: e.tensor_tensor(out=o_[:], in0=o_[:], in1=xb_[:], op=ALU.add), reads=[obk, xbk], writes=[obk])
                    op("act", lambda e, o_=o_: e.activation(out=fj[:], in_=o_[:], func=AF.Square, accum_out=f6[:, 0:1]), reads=[obk], writes=["fj", "f6"])
                    rstd_from_ss(f6[:, 0:1], 1024, f6[:, 1:2], f6[:, 2:3], ["f6"])
                    op("dve", lambda e, o_=o_: e.scalar_tensor_tensor(out=o_[:], in0=o_[:], scalar=f6[:, 1:2], in1=nfin[:], op0=ALU.mult, op1=ALU.mult),
                       reads=[obk, "f6", "nfin"], writes=[obk])
                    op("sp", lambda e, o_=o_, tsl=tsl: e.dma_start(out=y_out[tsl, :], in_=o_[:]), reads=[obk], dma=True, is_out=True)
        P.enabled = True
        if debug:
            P.barrier()
            for nm_, src_ in [("hT_d", hT_d), ("ropeC", ropeC), ("ropeS", ropeS), ("yaT_d", yaT_d), ("ypre_d", ypre_d), ("ps_d", ps_d), ("x1_d", x1_d), ("modrow", modrow)]:
                dst_ = nc.dram_tensor("dbg_" + nm_, list(src_.shape), src_.dtype, kind="ExternalOutput").ap()
                op("sp", lambda e, dst_=dst_, src_=src_: e.dma_start(out=dst_, in_=src_), dma=True, is_out=True)
        P.emit()
    return nc


def _prep_inputs(inp, n_exp=NEXP):
    f = lambda a: np.ascontiguousarray(np.asarray(a, dtype=np.float32))
    x = f(inp["x"])
    c = f(inp["c"])
    pos = np.asarray(inp["positions"]).astype(np.int32)
    w_in = f(inp["w_in"])[0]
    sizes = [1024, 1024, 1024, 2048, 3072, 32, 2048]
    offs = np.cumsum([0] + sizes)
    wq, wk, wv, wz, wxbc, wdt, wg = [w_in[:, offs[i]:offs[i + 1]] for i in range(7)]
    perm64 = np.arange(64)
    perm64[:8] = np.arange(8, 16)
    perm64[8:16] = np.arange(0, 8)
    perm128 = np.concatenate([perm64, 64 + perm64])
    wqk = np.empty((8, 5, 1024, 128), np.float32)
    for h in range(8):
        qh = wq[:, h * 128:(h + 1) * 128]
        kh = wk[:, h * 128:(h + 1) * 128]
        wqk[h, 0] = qh
        wqk[h, 1] = qh[:, perm128]
        wqk[h, 2] = kh
        wqk[h, 3] = kh[:, perm128]
        wqk[h, 4] = wv[:, h * 128:(h + 1) * 128]
    conv_w = f(inp["conv_w"])[0]
    conv_b = f(inp["conv_b"])[0]
    convw = np.ascontiguousarray(conv_w.T.reshape(24, 128, 4).transpose(1, 0, 2))
    convb = np.ascontiguousarray(conv_b.reshape(24, 128).T)
    weg = np.concatenate([f(inp["w_exp_gate"])[0][:n_exp], f(inp["w_sh_gate"])], axis=0)
    weu = np.concatenate([f(inp["w_exp_up"])[0][:n_exp], f(inp["w_sh_up"])], axis=0)
    wed = np.concatenate([f(inp["w_exp_down"])[0][:n_exp], f(inp["w_sh_down"])], axis=0)
    ident = np.eye(128, dtype=np.float32)
    tri = np.triu(np.ones((128, 128), np.float32))
    ones = np.ones((128, 128), np.float32)
    cmask = np.zeros((4, 128, 512), np.float32)
    kk = np.arange(128)[:, None]
    qq = np.arange(512)[None, :]
    for d in range(4):
        cmask[d] = (d * 128 + kk <= qq)
    invf = np.zeros((128, 2), np.float32)
    inv_freq = (1.0 / (np.float32(500000.0) ** (np.arange(0, 16, 2, dtype=np.float32) / np.float32(16)))).astype(np.float32)
    for p in range(128):
        dd = p % 64
        if dd < 16:
            invf[p, 0] = inv_freq[dd % 8]
            invf[p, 1] = -1.0 if dd < 8 else 1.0
    shared = {
        "w_ada": f(inp["w_ada"])[0], "b_ada": f(inp["b_ada"]), "norm_mix": f(inp["norm_mix"]), "norm_ffn": f(inp["norm_ffn"]),
        "norm_final": f(inp["norm_final"]).reshape(1, 1024), "wqk": wqk, "wz": np.ascontiguousarray(wz), "wxbc": np.ascontiguousarray(wxbc),
        "wdt": np.ascontiguousarray(wdt), "wg": np.ascontiguousarray(wg), "b_gate": f(inp["b_gate"]),
        "lamv": np.stack([f(inp["lambda_q1"])[0], f(inp["lambda_k1"])[0], f(inp["lambda_q2"])[0], f(inp["lambda_k2"])[0]]),
        "ahn": f(inp["attn_head_norm"]), "convw": convw, "convb": convb, "dt_bias": f(inp["dt_bias"]), "a_log": f(inp["a_log"]),
        "d_skip": f(inp["d_skip"]), "ssd_norm": f(inp["ssd_norm"]), "wba": f(inp["w_branch_attn"])[0], "wbs": f(inp["w_branch_ssd"])[0],
        "wo": f(inp["w_out"])[0], "wr": f(inp["w_router"])[0], "rbias": f(inp["router_bias"]), "weg": weg, "weu": weu, "wed": wed,
        "ident": ident, "tri": tri, "ones": ones, "cmask": cmask, "invf": invf,
    }
    in_maps = []
    for core in range(8):
        b, j = core // 4, core % 4
        npad = 6144 - 2048 * j
        xl = np.zeros((8192, 1024), np.float32)
        xl[npad:] = x[b, :2048 * (j + 1)]
        valid = np.zeros(8192, np.float32)
        valid[npad:] = 1.0
        posl = np.zeros(8192, np.int32)
        posl[npad:] = pos[b, :2048 * (j + 1)]
        m = dict(shared)
        m["xl"] = xl
        m["validb"] = np.ascontiguousarray(np.broadcast_to(valid[None, :], (128, 8192)))
        m["valid_tm"] = np.ascontiguousarray(valid.reshape(64, 128).T)
        m["posb"] = np.ascontiguousarray(np.broadcast_to(posl[None, :], (128, 8192)))
        m["c_col"] = np.ascontiguousarray(c[b].reshape(8, 128).T)
        in_maps.append(m)
    return in_maps


_NC_CACHE = {}


def kernel(**inputs):
    in_maps = _prep_inputs(inputs)
    if "nc" not in _NC_CACHE:
        _NC_CACHE["nc"] = build_nc()
    nc = _NC_CACHE["nc"]
    res = run_bass_kernel_spmd(nc, in_maps, core_ids=list(range(8)))
    out = np.empty((2, 8192, 1024), np.float32)
    for core in range(8):
        b, j = core // 4, core % 4
        out[b, 2048 * j:2048 * (j + 1)] = res.results[core]["y_out"]
    return out
```
